# Optimizing a Trainium2 kernel written in Bass

```python
import jax, jax.numpy as jnp
from jax import lax
import numpy as np

D_MODEL = 1024
BATCH = 8
SEQ = 4096
DEPTH = 4

GRID_W = 64
CTX_LEN = 256
EPS = 1e-6

N_MIXERS = 3
POOL_SLOT, ATTN_SLOT, SGU_SLOT = 0, 1, 2
N_POOL_LAYERS = len(range(POOL_SLOT, DEPTH, N_MIXERS))
N_ATTN_LAYERS = len(range(ATTN_SLOT, DEPTH, N_MIXERS))
N_SGU_LAYERS = len(range(SGU_SLOT, DEPTH, N_MIXERS))
CTX_LAST_READER = max([i for i in range(DEPTH) if i % N_MIXERS == ATTN_SLOT], default=-1)

POOL_WINDOWS = (2, 4, 8, 16)
POOL_GROUPS = len(POOL_WINDOWS)
POOL_CH = D_MODEL // POOL_GROUPS

HEAD_DIM = 64
N_Q_HEADS = D_MODEL // HEAD_DIM
N_KV_HEADS = 4
Q_PER_KV = N_Q_HEADS // N_KV_HEADS
Q_DIM = N_Q_HEADS * HEAD_DIM
KV_DIM = N_KV_HEADS * HEAD_DIM
QKV_DIM = Q_DIM + 2 * KV_DIM
ROPE_THETA = 10000.0
ROPE_AXIS_DIM = HEAD_DIM // 2
Q_BLOCK = 128

CHUNK = 128
SGU_WIDTH = D_MODEL
SGU_CH = 128
SGU_GROUPS = SGU_WIDTH // SGU_CH

N_EXPERT_GROUPS = 4
EXPERTS_PER_GROUP = 4
N_EXPERTS = N_EXPERT_GROUPS * EXPERTS_PER_GROUP
TOPK_IN_GROUP = 2
D_EXPERT = 256

kernel_name = "hybrid_pool_gqa_gmlp_hmoe_dit"


def rmsnorm(x, g):
    xf = x.astype(jnp.float32)
    y = xf * lax.rsqrt(jnp.mean(xf * xf, axis=-1, keepdims=True) + EPS)
    return (y * g.astype(jnp.float32)).astype(x.dtype)


def layernorm(x, g, b):
    xf = x.astype(jnp.float32)
    mu = jnp.mean(xf, axis=-1, keepdims=True)
    xc = xf - mu
    var = jnp.mean(xc * xc, axis=-1, keepdims=True)
    return (xc * lax.rsqrt(var + EPS) * g.astype(jnp.float32) + b.astype(jnp.float32)).astype(x.dtype)


def adaln(cvec, w_mod, b_mod):
    m = jax.nn.silu(cvec) @ w_mod + b_mod
    m = m.reshape(cvec.shape[:-1] + (6, 1, D_MODEL))
    return tuple(m[..., k, :, :] for k in range(6))


def modulate(x, g, shift, scale):
    return rmsnorm(x, g) * (1.0 + scale) + shift


def rope_tables(rows):
    row = jnp.repeat(jnp.arange(rows), GRID_W)
    col = jnp.tile(jnp.arange(GRID_W), rows)
    inv = ROPE_THETA ** (-jnp.arange(0, ROPE_AXIS_DIM, 2, dtype=jnp.float32) / ROPE_AXIS_DIM)
    ang = jnp.stack([row, col], axis=-1).astype(jnp.float32)[..., None] * inv
    return jnp.cos(ang), jnp.sin(ang)


def apply_rope(x, cos, sin):
    B, n, H, _ = x.shape
    xr = x.astype(jnp.float32).reshape(B, n, H, 2, 2, ROPE_AXIS_DIM // 2)
    x1, x2 = xr[..., 0, :], xr[..., 1, :]
    c, s = cos[:, None], sin[:, None]
    out = jnp.stack([x1 * c - x2 * s, x2 * c + x1 * s], axis=-2)
    return out.reshape(x.shape).astype(x.dtype)


def pool_mixer(h, w_pool, b_pool, ls):
    B, n, _ = h.shape
    t = jnp.arange(n)
    hf = h.astype(jnp.float32)
    outs = []
    for g, w in enumerate(POOL_WINDOWS):
        xg = hf[..., g * POOL_CH:(g + 1) * POOL_CH]
        cs = jnp.concatenate([jnp.zeros_like(xg[:, :1]), jnp.cumsum(xg, axis=1)], axis=1)
        lo = jnp.clip(t - w // 2, 0, n)
        hi = jnp.clip(t + w // 2, 0, n)
        mean = (cs[:, hi] - cs[:, lo]) / (hi - lo).astype(jnp.float32)[:, None]
        outs.append(mean - xg)
    p = jnp.stack(outs, axis=2).astype(h.dtype)
    y = jnp.einsum('bngc,gcd->bngd', p, w_pool) + b_pool
    return y.reshape(h.shape) * ls


def attn_mixer(h_lat, h_ctx, w_qkv, q_g, k_g, w_o, cos, sin, ctx_out):
    B, S, _ = h_lat.shape
    L = h_ctx.shape[1]
    scale = HEAD_DIM ** -0.5
    qkv = h_lat @ w_qkv
    q = rmsnorm(qkv[..., :Q_DIM].reshape(B, S, N_Q_HEADS, HEAD_DIM), q_g)
    k = rmsnorm(qkv[..., Q_DIM:Q_DIM + KV_DIM].reshape(B, S, N_KV_HEADS, HEAD_DIM), k_g)
    v = qkv[..., Q_DIM + KV_DIM:].reshape(B, S, N_KV_HEADS, HEAD_DIM)
    q = apply_rope(q, cos, sin)
    k = apply_rope(k, cos, sin)
    kv_c = h_ctx @ w_qkv[:, Q_DIM:]
    k_c = rmsnorm(kv_c[..., :KV_DIM].reshape(B, L, N_KV_HEADS, HEAD_DIM), k_g)
    v_c = kv_c[..., KV_DIM:].reshape(B, L, N_KV_HEADS, HEAD_DIM)
    k_all = jnp.concatenate([k_c, k], axis=1)
    v_all = jnp.concatenate([v_c, v], axis=1)
    qb = (q * scale).reshape(B, S // Q_BLOCK, Q_BLOCK, N_KV_HEADS, Q_PER_KV, HEAD_DIM)
    qb = jnp.moveaxis(qb, 1, 0)

    def block(q_blk):
        s = jnp.einsum('bqhgd,bkhd->bhgqk', q_blk, k_all).astype(jnp.float32)
        p = jax.nn.softmax(s, axis=-1).astype(v_all.dtype)
        return jnp.einsum('bhgqk,bkhd->bqhgd', p, v_all)

    o = jnp.moveaxis(lax.map(block, qb), 0, 1).reshape(B, S, Q_DIM)
    y_lat = o @ w_o
    y_ctx = None
    if ctx_out:
        q_c = rmsnorm((h_ctx @ w_qkv[:, :Q_DIM]).reshape(B, L, N_Q_HEADS, HEAD_DIM), q_g)
        q_c = (q_c * scale).reshape(B, L, N_KV_HEADS, Q_PER_KV, HEAD_DIM)
        s_c = jnp.einsum('bqhgd,bkhd->bhgqk', q_c, k_c).astype(jnp.float32)
        p_c = jax.nn.softmax(s_c, axis=-1).astype(v_c.dtype)
        y_ctx = jnp.einsum('bhgqk,bkhd->bqhgd', p_c, v_c).reshape(B, L, Q_DIM) @ w_o
    return y_lat, y_ctx


def sgu_mixer(h, w_in, b_in, ln_g, ln_b, w_s, b_s, w_out):
    B, n, _ = h.shape
    z = jax.nn.gelu(h @ w_in + b_in)
    u, v = z[..., :SGU_WIDTH], z[..., SGU_WIDTH:]
    v = layernorm(v, ln_g, ln_b)
    vc = v.reshape(B, n // CHUNK, CHUNK, SGU_GROUPS, SGU_CH)
    sv = jnp.einsum('gpq,bcqgk->bcpgk', w_s, vc) + b_s.T[:, :, None]
    return (u * sv.reshape(B, n, SGU_WIDTH)) @ w_out


def hier_moe(h, w_rg, b_rg, w_re, b_re, w_gate, w_up, w_down):
    shp = h.shape
    hf = h.reshape(-1, D_MODEL)
    n = hf.shape[0]
    g_logits = (hf @ w_rg).astype(jnp.float32) + b_rg
    g_prob = jax.nn.softmax(g_logits, axis=-1)
    g_sel = jnp.argmax(g_logits, axis=-1)
    g_w = jnp.take_along_axis(g_prob, g_sel[:, None], axis=-1)
    e_logits = ((hf @ w_re).astype(jnp.float32) + b_re).reshape(n, N_EXPERT_GROUPS, EXPERTS_PER_GROUP)
    e_in = jnp.take_along_axis(e_logits, g_sel[:, None, None], axis=1)[:, 0]
    top_v, top_i = lax.top_k(e_in, TOPK_IN_GROUP)
    e_w = jax.nn.softmax(top_v, axis=-1) * g_w
    expert_id = g_sel[:, None] * EXPERTS_PER_GROUP + top_i
    gates = jnp.sum(jax.nn.one_hot(expert_id, N_EXPERTS, dtype=jnp.float32) * e_w[..., None], axis=1)
    a = jnp.einsum('nd,edf->nef', hf, w_gate)
    b = jnp.einsum('nd,edf->nef', hf, w_up)
    hid = jax.nn.silu(a) * b * gates.astype(h.dtype)[..., None]
    return jnp.einsum('nef,efd->nd', hid, w_down).reshape(shp)


def setup_inputs(seed: int = 0) -> dict:
    key = jax.random.key(seed)
    ks = iter(jax.random.split(key, 40))
    D = D_MODEL

    def nrm(shape, scale):
        return jax.random.normal(next(ks), shape, jnp.float32) * scale

    return {
        "x": nrm((BATCH, SEQ, D), 1.0),
        "c": nrm((BATCH, D), 1.0),
        "ctx": nrm((BATCH, CTX_LEN, D), 1.0),
        "c_ctx": nrm((D,), 1.0),
        "w_mod": nrm((DEPTH, D, 6 * D), 0.5 * D ** -0.5),
        "b_mod": nrm((DEPTH, 6 * D), 0.02),
        "norm1_g": 1.0 + nrm((DEPTH, D), 0.02),
        "norm2_g": 1.0 + nrm((DEPTH, D), 0.02),
        "pool_w": nrm((N_POOL_LAYERS, POOL_GROUPS, POOL_CH, POOL_CH), POOL_CH ** -0.5),
        "pool_b": nrm((N_POOL_LAYERS, POOL_GROUPS, POOL_CH), 0.02),
        "pool_ls": 1.0 + nrm((N_POOL_LAYERS, D), 0.02),
        "attn_wqkv": nrm((N_ATTN_LAYERS, D, QKV_DIM), D ** -0.5),
        "attn_qg": 1.0 + nrm((N_ATTN_LAYERS, HEAD_DIM), 0.02),
        "attn_kg": 1.0 + nrm((N_ATTN_LAYERS, HEAD_DIM), 0.02),
        "attn_wo": nrm((N_ATTN_LAYERS, Q_DIM, D), Q_DIM ** -0.5),
        "sgu_win": nrm((N_SGU_LAYERS, D, 2 * SGU_WIDTH), D ** -0.5),
        "sgu_bin": nrm((N_SGU_LAYERS, 2 * SGU_WIDTH), 0.02),
        "sgu_lng": 1.0 + nrm((N_SGU_LAYERS, SGU_WIDTH), 0.02),
        "sgu_lnb": nrm((N_SGU_LAYERS, SGU_WIDTH), 0.02),
        "sgu_ws": nrm((N_SGU_LAYERS, SGU_GROUPS, CHUNK, CHUNK), CHUNK ** -0.5),
        "sgu_bs": 1.0 + nrm((N_SGU_LAYERS, SGU_GROUPS, CHUNK), 0.02),
        "sgu_wout": nrm((N_SGU_LAYERS, SGU_WIDTH, D), SGU_WIDTH ** -0.5),
        "moe_wrg": nrm((DEPTH, D, N_EXPERT_GROUPS), D ** -0.5),
        "moe_brg": nrm((DEPTH, N_EXPERT_GROUPS), 0.01),
        "moe_wre": nrm((DEPTH, D, N_EXPERTS), D ** -0.5),
        "moe_bre": nrm((DEPTH, N_EXPERTS), 0.01),
        "moe_wg": nrm((DEPTH, N_EXPERTS, D, D_EXPERT), D ** -0.5),
        "moe_wu": nrm((DEPTH, N_EXPERTS, D, D_EXPERT), D ** -0.5),
        "moe_wd": nrm((DEPTH, N_EXPERTS, D_EXPERT, D), D_EXPERT ** -0.5),
    }


def reference(x, c, ctx, c_ctx, w_mod, b_mod, norm1_g, norm2_g, pool_w, pool_b, pool_ls,
              attn_wqkv, attn_qg, attn_kg, attn_wo, sgu_win, sgu_bin, sgu_lng, sgu_lnb, sgu_ws, sgu_bs,
              sgu_wout, moe_wrg, moe_brg, moe_wre, moe_bre, moe_wg, moe_wu, moe_wd):
    rows = x.shape[1] // GRID_W
    cos, sin = rope_tables(rows)
    s = ctx
    for i in range(DEPTH):
        kind = i % N_MIXERS
        j = i // N_MIXERS
        ctx_in = i <= CTX_LAST_READER
        ctx_upd = i < CTX_LAST_READER
        sh1, sc1, g1, sh2, sc2, g2 = adaln(c, w_mod[i], b_mod[i])
        a_lat = modulate(x, norm1_g[i], sh1, sc1)
        if ctx_in:
            csh1, csc1, cg1, csh2, csc2, cg2 = adaln(c_ctx, w_mod[i], b_mod[i])
            a_ctx = modulate(s, norm1_g[i], csh1, csc1)
        if kind == POOL_SLOT:
            y_lat = pool_mixer(a_lat, pool_w[j], pool_b[j], pool_ls[j])
            if ctx_upd:
                y_ctx = pool_mixer(a_ctx, pool_w[j], pool_b[j], pool_ls[j])
        elif kind == ATTN_SLOT:
            y_lat, y_ctx = attn_mixer(a_lat, a_ctx, attn_wqkv[j], attn_qg[j], attn_kg[j], attn_wo[j],
                                      cos, sin, ctx_upd)
        else:
            y_lat = sgu_mixer(a_lat, sgu_win[j], sgu_bin[j], sgu_lng[j], sgu_lnb[j], sgu_ws[j], sgu_bs[j],
                              sgu_wout[j])
            if ctx_upd:
                y_ctx = sgu_mixer(a_ctx, sgu_win[j], sgu_bin[j], sgu_lng[j], sgu_lnb[j], sgu_ws[j],
                                  sgu_bs[j], sgu_wout[j])
        x = x + g1 * y_lat
        x = x + g2 * hier_moe(modulate(x, norm2_g[i], sh2, sc2), moe_wrg[i], moe_brg[i], moe_wre[i],
                              moe_bre[i], moe_wg[i], moe_wu[i], moe_wd[i])
        if ctx_upd:
            s = s + cg1 * y_ctx
            s = s + cg2 * hier_moe(modulate(s, norm2_g[i], csh2, csc2), moe_wrg[i], moe_brg[i],
                                   moe_wre[i], moe_bre[i], moe_wg[i], moe_wu[i], moe_wd[i])
    return x
```

```python
import numpy as np
import concourse.bass as bass
import concourse.mybir as mybir
from concourse.bass_utils import run_bass_kernel_spmd

F32, BF16 = mybir.dt.float32, mybir.dt.bfloat16
AF = mybir.ActivationFunctionType
ALU = mybir.AluOpType
AX = mybir.AxisListType

D = 1024
SEQ = 4096
CTXN = 256
DEPTH = 4
NCH = 8
EPS = 1e-6
NEXP = 16


class Prog:
    NDSEM = 12

    def __init__(self, nc):
        self.nc = nc
        self.ops = []
        self.eng = {"pe": nc.tensor, "act": nc.scalar, "dve": nc.vector, "pool": nc.gpsimd, "sp": nc.sync}

    def op(self, eng, fn, reads=(), writes=(), dma=False, final=False):
        o = dict(eng=eng, fn=fn, reads=tuple(reads), writes=tuple(writes), dma=dma, final=final, barrier=False)
        if getattr(self, "_cap", None) is not None:
            self._cap.append(o)
        else:
            self.ops.append(o)

    def begin_capture(self):
        self._cap = []

    def end_capture(self):
        c, self._cap = self._cap, None
        return c

    def emit(self, o):
        self.ops.append(o)

    def barrier(self):
        self.ops.append(dict(barrier=True))

    def finalize(self):
        nc = self.nc
        ops = []
        last_on = {}
        dmas = []
        pending = {}
        for o in self.ops:
            if o["barrier"]:
                bd = set(last_on.values()) | set(dmas)
                dmas = []
                for e in self.eng:
                    pending[e] = pending.get(e, set()) | bd
                continue
            idx = len(ops)
            o["bdeps"] = pending.pop(o["eng"], set())
            if o["dma"]:
                dmas.append(idx)
            else:
                last_on[o["eng"]] = idx
            ops.append(o)
        last_w, readers = {}, {}
        for idx, o in enumerate(ops):
            deps = set(o["bdeps"])
            for k in o["reads"]:
                if k in last_w:
                    deps.add(last_w[k])
            for k in o["writes"]:
                if k in last_w:
                    deps.add(last_w[k])
                deps.update(readers.get(k, ()))
            deps.discard(idx)
            if o["eng"] == "pe" and not o["dma"]:
                deps = {d for d in deps if not (ops[d]["eng"] == "pe" and not ops[d]["dma"])}
            o["deps"] = deps
            for k in o["reads"]:
                readers.setdefault(k, []).append(idx)
            for k in o["writes"]:
                last_w[k] = idx
                readers[k] = []
        for o in ops:
            o["signal"] = False
        for o in ops:
            for d in o["deps"]:
                ops[d]["signal"] = True
        csem = {e: nc.alloc_semaphore("cs_" + e) for e in ("pe", "act", "dve", "pool")}
        dsem = {q: [nc.alloc_semaphore("ds_%s%d" % (q, j)) for j in range(self.NDSEM)] for q in ("sp", "pool", "act")}
        cnt = {e: 0 for e in csem}
        dcnt = {q: 0 for q in dsem}
        for o in ops:
            if o["dma"]:
                q = o["eng"]
                j = dcnt[q]
                dcnt[q] += 1
                o["dsem"] = dsem[q][j % self.NDSEM]
                o["dval"] = 16 * (j // self.NDSEM + 1)
                o["dprev"] = 16 * (j // self.NDSEM)
            elif o["signal"]:
                cnt[o["eng"]] += 1
                o["sval"] = cnt[o["eng"]]
        waited = {e: {} for e in self.eng}
        finals = []
        for o in ops:
            e = o["eng"]
            E = self.eng[e]
            need = {}
            for d in o["deps"]:
                p = ops[d]
                if p["dma"]:
                    s, v = p["dsem"], p["dval"]
                else:
                    s, v = csem[p["eng"]], p["sval"]
                if need.get(s, 0) < v:
                    need[s] = v
            if o["dma"] and o["dprev"] > 0:
                s = o["dsem"]
                if need.get(s, 0) < o["dprev"]:
                    need[s] = o["dprev"]
            for s, v in need.items():
                if waited[e].get(s, 0) < v:
                    E.wait_ge(s, v)
                    waited[e][s] = v
            ins = o["fn"](E)
            if o["dma"]:
                ins.then_inc(o["dsem"], 16)
                if o["final"]:
                    finals.append((o["dsem"], o["dval"]))
            elif o["signal"]:
                ins.then_inc(csem[e], 1)
        for s, v in finals:
            nc.sync.wait_ge(s, v)


def _keys(name, *ranges):
    out = [(name,)]
    for r in ranges:
        out = [k + (v,) for k in out for v in r]
    return out


def _tiles(t0, n, step=512):
    return range(t0 // step, (t0 + n - 1) // step + 1)


class Builder:
    def __init__(self, nc, layers, phases=None, store_s=True):
        self.nc = nc
        self.store_s = store_s
        self.P = Prog(nc)
        self.layers = list(layers)
        self.phases = phases
        L = len(self.layers)
        self.L = L
        dr = lambda name, shape: nc.dram_tensor(name, list(shape), F32, kind="ExternalInput").ap()
        self.d_xT = dr("xT", [NCH, 128, SEQ])
        self.d_sT = dr("sT", [NCH, 128, CTXN])
        self.d_cc = dr("ccT", [128, NCH, 2])
        self.d_consts = dr("consts", [128, 256])
        self.d_wmod = dr("wmod", [L, 12, 128, NCH, 512])
        self.d_bmod = dr("bmod", [128, L, 48])
        self.d_ng = dr("ng", [128, L, 2, NCH])
        self.d_wr = dr("wr", [128, L, NCH, 20])
        self.d_br = dr("br", [128, L, 20])
        self.d_wgu = dr("wgu", [L, NEXP, 128, NCH, 512])
        self.d_wd = dr("wd", [L, NEXP, 128, 2, 1024])
        self.d_pw = dr("pw", [2, 128, 4, 2, 256])
        self.d_pv = dr("pv", [128, 2, 2, NCH])
        self.d_wq = dr("wq", [NCH, 128, NCH, 128])
        self.d_wkv = dr("wkv", [128, NCH, 512])
        self.d_wo = dr("wo", [4, 128, 2, 1024])
        self.d_rope = dr("rope", [2, 128, SEQ])
        self.d_av = dr("av", [128, 2])
        self.d_winu = dr("winu", [NCH, 128, NCH, 128])
        self.d_winv = dr("winv", [128, NCH, 1024])
        self.d_wout = dr("wout", [NCH, 128, NCH, 128])
        self.d_sgv = dr("sgv", [128, 2, NCH])
        self.d_binv = dr("binv", [1, 1024])
        self.d_lnb = dr("lnb", [128, 1024])
        self.d_bsb = dr("bsb", [128, 8, 128])
        self.d_wsT = dr("wsT", [128, 8, 128])
        self.d_out = nc.dram_tensor("yT", [NCH, 128, SEQ], F32, kind="ExternalOutput").ap()
        if store_s:
            self.d_sout = nc.dram_tensor("soT", [NCH, 128, CTXN], F32, kind="ExternalOutput").ap()
        self.alloc()

    def alloc(self):
        nc = self.nc
        L = self.L
        sb = lambda name, shape, dt=F32: nc.alloc_sbuf_tensor(name, list(shape), dt).ap()
        self.xT = sb("xT_sb", [128, NCH, SEQ])
        self.sT = sb("sT_sb", [128, NCH, CTXN])
        self.consts = sb("consts_sb", [128, 256])
        self.ident = self.consts[:, 0:128]
        self.identb = sb("identb", [128, 128], BF16)
        self.onesb = sb("onesb", [128, 128], BF16)
        self.selb = sb("selb", [32, NEXP], BF16)
        self.epst = sb("epst", [128, 1])
        self.ccT = sb("ccT_sb", [128, NCH, 2])
        self.scT = sb("scT", [128, NCH, 2])
        self.mods = sb("mods", [128, L, 48, 2])
        self.ng = sb("ng_sb", [128, L, 2, NCH])
        self.gs = sb("gs", [128, L, 2, 2, NCH])
        self.wr = sb("wr_sb", [128, NCH, 20])
        self.br = sb("br_sb", [128, 20])
        self.wrb = sb("wrb", [128, 2, NCH, 20], BF16)
        self.pv = sb("pv_sb", [128, 2, 2, NCH])
        self.pcoef = sb("pcoef", [128, 2, NCH])
        self.rt = sb("rt", [128, 512])
        self.av = sb("av_sb", [128, 2])
        ARENA = 16384
        self.arena = sb("arena", [128, ARENA])
        self.ARENA = ARENA
        ps = lambda name: nc.alloc_psum_tensor(name, [128, 512], F32).ap()
        self.psA = [ps("psA0"), ps("psA1")]
        self.psB = [ps("psB0"), ps("psB1")]
        self.psG = ps("psG")
        self.psO = [ps("psO0"), ps("psO1")]
        self.psM = ps("psM")

    def dbg(self, name, ap, keys):
        if not getattr(self, "debug", False):
            return
        shp = list(ap.shape)
        d = self.nc.dram_tensor("dbg_" + name, shp, F32, kind="ExternalOutput").ap()
        self.P.op("pool", lambda E: E.dma_start(out=d, in_=ap), reads=keys, dma=True, final=True)

    def carve(self, off, nwords, dt=F32, shape=None):
        assert off + nwords <= self.ARENA, (off, nwords)
        a = self.arena[:, off:off + nwords]
        if dt == BF16:
            a = a.bitcast(BF16)
        if shape is not None:
            names = " ".join("a%d" % i for i in range(len(shape)))
            kw = {"a%d" % i: s for i, s in enumerate(shape)}
            a = a.rearrange("p (%s) -> p %s" % (names, names), **kw)
        return a

    def S(self, stream):
        return (self.xT, SEQ) if stream == "x" else (self.sT, CTXN)

    def skeys(self, stream, chunks, t0, n):
        return _keys(stream, chunks, _tiles(t0, n))

    def prologue(self):
        P, nc = self.P, self.nc
        L = self.L
        self.bmod = self.carve(8192, L * 48, F32, [L, 48])
        P.op("sp", lambda E: E.dma_start(out=self.consts, in_=self.d_consts), writes=[("consts",)], dma=True)
        P.op("sp", lambda E: E.dma_start(out=self.ccT, in_=self.d_cc), writes=[("ccT",)], dma=True)
        P.op("sp", lambda E: E.dma_start(out=self.bmod, in_=self.d_bmod), writes=[("bmod",)], dma=True)
        P.op("sp", lambda E: E.dma_start(out=self.ng, in_=self.d_ng), writes=[("ng",)], dma=True)
        P.op("sp", lambda E: E.dma_start(out=self.pv, in_=self.d_pv), writes=[("pv",)], dma=True)
        for c in range(NCH):
            for h in range(2):
                P.op("sp", lambda E, c=c, h=h: E.dma_start(out=self.xT[:, c, h * 2048:(h + 1) * 2048],
                                                           in_=self.d_xT[c, :, h * 2048:(h + 1) * 2048]),
                     writes=self.skeys("x", [c], h * 2048, 2048), dma=True)
            P.op("sp", lambda E, c=c: E.dma_start(out=self.sT[:, c, :], in_=self.d_sT[c]),
                 writes=self.skeys("s", [c], 0, CTXN), dma=True)
        P.op("dve", lambda E: E.tensor_copy(out=self.identb, in_=self.ident), reads=[("consts",)], writes=[("identb",)])
        P.op("pool", lambda E: E.memset(self.onesb, 1.0 / D), writes=[("onesb",)])
        P.op("pool", lambda E: E.memset(self.epst, EPS), writes=[("epst",)])
        P.op("dve", lambda E: E.tensor_copy(out=self.selb, in_=self.consts[0:32, 128:144]),
             reads=[("consts",)], writes=[("selb",)])
        scTb = self.carve(12544, 8, BF16, [NCH, 2])
        P.op("act", lambda E: E.activation(out=scTb, in_=self.ccT, func=AF.Silu), reads=[("ccT",)], writes=[("scT",)])
        wm = [self.carve(0, 4096, F32, [NCH, 512]), self.carve(4096, 4096, F32, [NCH, 512])]
        wmb = [self.carve(8448, 2048, BF16, [NCH, 512]), self.carve(10496, 2048, BF16, [NCH, 512])]
        n = 0
        for l in range(L):
            for blk in range(12):
                buf = wm[n % 2]
                bufb = wmb[n % 2]
                for kh in range(2):
                    P.op("sp", lambda E, l=l, blk=blk, buf=buf, kh=kh: E.dma_start(out=buf[:, kh * 4:(kh + 1) * 4, :],
                                                                                    in_=self.d_wmod[l, blk, :, kh * 4:(kh + 1) * 4, :]),
                         writes=[("wm", n % 2, kh)], dma=True)
                    if kh == 0:
                        P.op("act", lambda E, buf=buf, bufb=bufb: E.activation(out=bufb[:, 0:4, :], in_=buf[:, 0:4, :], func=AF.Copy),
                             reads=[("wm", n % 2, 0)], writes=[("wmb", n % 2, 0)])
                    else:
                        P.op("dve", lambda E, buf=buf, bufb=bufb: E.tensor_copy(out=bufb[:, 4:8, :], in_=buf[:, 4:8, :]),
                             reads=[("wm", n % 2, 1)], writes=[("wmb", n % 2, 1)])

                def mm(E, bufb=bufb):
                    ins = None
                    for f in range(4):
                        for k in range(NCH):
                            ins = E.matmul(self.psM[:, 2 * f:2 * f + 2], lhsT=bufb[:, k, f * 128:(f + 1) * 128],
                                           rhs=scTb[:, k, :], start=(k == 0), stop=(k == NCH - 1))
                    return ins
                P.op("pe", mm, reads=[("wmb", n % 2, 0), ("wmb", n % 2, 1), ("scT",)], writes=[("psM",)])
                P.op("dve", lambda E, l=l, blk=blk: E.tensor_tensor(
                    out=self.mods[:, l, blk * 4:(blk + 1) * 4, :],
                    in0=self.psM[:, 0:8].rearrange("p (f j) -> p f j", j=2),
                    in1=self.bmod[:, l, blk * 4:(blk + 1) * 4].unsqueeze(2).broadcast_to([128, 4, 2]), op=ALU.add),
                    reads=[("psM",), ("bmod",)], writes=[("mods", l, blk)])
                n += 1
            for nn in range(2):
                for j in range(2):
                    q0 = 8 + 24 * nn
                    P.op("dve", lambda E, l=l, nn=nn, j=j, q0=q0: E.scalar_tensor_tensor(
                        out=self.gs[:, l, nn, j, :], in0=self.mods[:, l, q0:q0 + 8, j], scalar=1.0,
                        in1=self.ng[:, l, nn, :], op0=ALU.add, op1=ALU.mult),
                        reads=[("mods", l, b) for b in range(12)] + [("ng",)], writes=[("gs", l)])
        self.wm_keys = [("wm", 0), ("wm", 1)]

    def mod(self, l, s, j):
        return self.mods[:, l, s * 8:(s + 1) * 8, j]

    def modkeys(self, l):
        return [("mods", l, b) for b in range(12)] + [("gs", l)]

    def rstd_tile(self, stream, t0, n, out_ap, out_key, sq, sqkeys, ps=None, ps_key=None, all_pool=False):
        P = self.P
        src, _ = self.S(stream)
        if ps is None:
            ps, ps_key = self.psM, ("psM",)
        for c in range(NCH):
            if c % 2 == 0 or all_pool:
                P.op("pool", lambda E, c=c: E.tensor_tensor(out=sq[c % 2][:, :n], in0=src[:, c, t0:t0 + n],
                                                             in1=src[:, c, t0:t0 + n], op=ALU.mult),
                     reads=self.skeys(stream, [c], t0, n), writes=[sqkeys[c % 2]])
            else:
                P.op("act", lambda E, c=c: E.activation(out=sq[c % 2][:, :n], in_=src[:, c, t0:t0 + n], func=AF.Square),
                     reads=self.skeys(stream, [c], t0, n), writes=[sqkeys[c % 2]])
            P.op("pe", lambda E, c=c: E.matmul(ps[:, :n], lhsT=self.onesb, rhs=sq[c % 2][:, :n],
                                               start=(c == 0), stop=(c == NCH - 1)),
                 reads=[sqkeys[c % 2], ("onesb",)], writes=[ps_key])
        P.op("act", lambda E: E.activation(out=out_ap, in_=ps[:, :n], func=AF.Ln, bias=self.epst, scale=1.0),
             reads=[ps_key, ("epst",)], writes=[out_key])
        P.op("act", lambda E: E.activation(out=out_ap, in_=out_ap, func=AF.Exp, scale=-0.5),
             reads=[out_key], writes=[out_key])

    def moe(self, l, streams):
        P = self.P
        big = (streams == "x" and self.layers[l] >= 1)
        NH = 2048 if big else 1536
        passes = []
        if "x" in streams:
            for T0 in range(0, SEQ, NH):
                NP = min(NH, SEQ - T0)
                passes.append([("x", T0 + k, min(512, NP - k), k) for k in range(0, NP, 512)])
            if "s" in streams:
                passes[-1].append(("s", 0, CTXN, 1024))
        else:
            passes.append([("s", 0, CTXN, 0)])

        def sv(stream):
            j = 0 if stream == "x" else 1
            return self.S(stream)[0], self.gs[:, l, 1, j, :], self.mod(l, 3, j), self.mod(l, 5, j)
        if big:
            h2T = self.carve(0, 8192, BF16, [NCH, 2048])
            sTf = self.sT.rearrange("p c t -> p (c t)")
            stg = [sTf[:, 0:1024], sTf[:, 1024:2048]]
        else:
            h2T = self.carve(0, 6144, BF16, [NCH, 1536])
            stg = [self.carve(6144, 1024), self.carve(7168, 1024)]
        wgu = [self.carve(8192, 2048, BF16, [NCH, 512]), self.carve(10240, 2048, BF16, [NCH, 512])]
        wdn = self.carve(12288, 1024, BF16, [2, 1024])
        gT2 = self.carve(13312, 1024, BF16, [2048])
        tmpf = [self.carve(14336, 512), self.carve(14848, 512)]
        sq = [self.carve(15360, 256, BF16), self.carve(15616, 256, BF16)]
        rstd = self.carve(15872, 512)
        sa = tmpf
        hid = [self.carve(15360, 512, BF16, [2, 512]), self.carve(15872, 512, BF16, [2, 512])]
        HK = [[("m_h0a",), ("m_h0b",)], [("m_h1",)]]
        mk = self.modkeys(l)
        rt = self.rt
        P.op("sp", lambda E: E.dma_start(out=self.wr, in_=self.d_wr[:, l]), writes=[("wr",)], dma=True)
        P.op("sp", lambda E: E.dma_start(out=self.br, in_=self.d_br[:, l]), writes=[("br",)], dma=True)
        wrf = self.wr.rearrange("p c k -> p (c k)")
        wtmp = self.rt[:, 0:160]
        P.op("dve", lambda E: E.tensor_copy(out=self.wrb[:, 0], in_=self.wr), reads=[("wr",)], writes=[("wrb",)])
        P.op("dve", lambda E: E.tensor_copy(out=wtmp, in_=self.wrb[:, 0].rearrange("p c k -> p (c k)")),
             reads=[("wrb",)], writes=[("rt",)])
        P.op("dve", lambda E: E.tensor_tensor(out=wtmp, in0=wrf, in1=wtmp, op=ALU.subtract),
             reads=[("wr",), ("rt",)], writes=[("rt",)])
        P.op("dve", lambda E: E.tensor_copy(out=self.wrb[:, 1].rearrange("p c k -> p (c k)"), in_=wtmp),
             reads=[("rt",)], writes=[("wrb",)])
        for pi, tiles in enumerate(passes):
            nt = len(tiles)
            seq = []

            def wgu_chunks(e):
                return [(self.d_wgu[l, e, :, 2 * q:2 * q + 2, :], wgu[e % 2][:, 2 * q:2 * q + 2, :], ("wgu", e % 2, q), True)
                        for q in range(4)]

            def wd_chunks(e):
                return [(self.d_wd[l, e, :, q, :], wdn[:, q, :], ("wdn", q), False) for q in range(2)]
            seq += wgu_chunks(0)
            for e in range(NEXP):
                seq += wd_chunks(e)
                if e + 1 < NEXP:
                    seq += wgu_chunks(e + 1)
            cnt = dict(d=0, c=0)

            def issue_dma():
                i = cnt["d"]
                if i >= len(seq):
                    return
                cnt["d"] += 1
                srcap, _, _, two = seq[i]
                sb_ = stg[i % 2]
                dst = sb_.rearrange("p (a b) -> p a b", a=2) if two else sb_
                P.op("sp", lambda E: E.dma_start(out=dst, in_=srcap), writes=[("stg", i % 2)], dma=True)

            def emit_cast():
                i = cnt["c"]
                if i >= len(seq):
                    return
                cnt["c"] += 1
                _, dstap, dkey, two = seq[i]
                sb_ = stg[i % 2]
                sv = sb_.rearrange("p (a b) -> p a b", a=2) if two else sb_
                P.op("act", lambda E: E.activation(out=dstap, in_=sv, func=AF.Copy), reads=[("stg", i % 2)], writes=[dkey])
                issue_dma()
            issue_dma()
            issue_dma()
            for _ in range(4):
                emit_cast()
            def emit_rstd(tile):
                stream_, t0_, n_, _ = tile
                self.rstd_tile(stream_, t0_, n_, rstd[:, :n_], ("m_h1",), sq, [("m_h0a",), ("m_h0b",)])
            emit_rstd(tiles[0])
            for tidx, (stream, t0, n, lo) in enumerate(tiles):
                src, gs2, sh2, g2 = sv(stream)
                ns = n // 128
                for c in range(NCH):
                    tb = tmpf[c % 2]
                    P.op("dve", lambda E, c=c, tb=tb, t0=t0, n=n, src=src, gs2=gs2: E.scalar_tensor_tensor(
                        out=tb[:, :n], in0=src[:, c, t0:t0 + n], scalar=gs2[:, c:c + 1], in1=rstd[:, :n],
                        op0=ALU.mult, op1=ALU.mult),
                        reads=self.skeys(stream, [c], t0, n) + mk + [("m_h1",)], writes=[("m_sa", c % 2)])
                    P.op("act", lambda E, c=c, tb=tb, lo=lo, n=n, sh2=sh2: E.activation(out=h2T[:, c, lo:lo + n], in_=tb[:, :n], func=AF.Identity,
                                                                   bias=sh2[:, c:c + 1], scale=1.0),
                         reads=[("m_sa", c % 2)] + mk, writes=[("h2T", c, lo // 512)])

                def rmm(E, lo=lo, ns=ns):
                    ins = None
                    for s in range(ns):
                        for c in range(NCH):
                            for part in range(2):
                                ins = E.matmul(self.psG[:, s * 20:(s + 1) * 20],
                                               lhsT=h2T[:, c, lo + s * 128:lo + (s + 1) * 128],
                                               rhs=self.wrb[:, part, c, :], start=(c == 0 and part == 0),
                                               stop=(c == NCH - 1 and part == 1))
                    return ins
                P.op("pe", rmm, reads=[("h2T", c, lo // 512) for c in range(NCH)] + [("wrb",)], writes=[("psG",)])
                if tidx + 1 < nt:
                    emit_rstd(tiles[tidx + 1])
                self.routing(l, ns, gT2, lo)
                if lo == 0 and pi == 0:
                    self.dbg("lgs_" + stream, self.rt[:, 0:80], [("rt",)])
                    self.dbg("rt_" + stream, self.rt[:, 0:512], [("rt",)])
                    self.dbg("gT2_" + stream, gT2[0:32, 0:n], [("gT2", 0)])
                    self.dbg("h2T_" + stream, h2T[:, 0, 0:n], [("h2T", 0, 0)])
                    self.dbg("h2T7_" + stream, h2T[:, 7, 0:n], [("h2T", 7, 0)])
                    self.dbg("tmp0_" + stream, tmpf[0][:, :n], [("m_sa", 0)])
                    self.dbg("rstd_" + stream, rstd[:, :n], [("m_h1",)])
                    self.dbg("xin_" + stream, src[:, 0, 0:n], self.skeys(stream, [0], 0, n))
            obanks = [(self.psO[0], ("psO", 0)), (self.psO[1], ("psO", 1)), (self.psM, ("psM",))]
            obank = [0]
            rtmp = self.rt
            pend = None
            it = 0
            for e in range(NEXP):
                wb = e % 2
                for ti, (stream, t0, n, lo) in enumerate(tiles):
                    src, gs2, sh2, g2 = sv(stream)
                    hb = it % 2
                    for fc in range(2):
                        def gu(E, fc=fc, wb=wb, lo=lo, n=n):
                            ins = None
                            for half, pst in ((0, self.psA[fc]), (1, self.psB[fc])):
                                for k in range(NCH):
                                    col = half * 256 + fc * 128
                                    ins = E.matmul(pst[:, :n], lhsT=wgu[wb][:, k, col:col + 128],
                                                   rhs=h2T[:, k, lo:lo + n], start=(k == 0), stop=(k == NCH - 1))
                            return ins
                        if ti >= 1 and e + 1 < NEXP:
                            per = -(-4 // (2 * (nt - 1)))
                            for _ in range(per):
                                if cnt["c"] < 4 + 6 * e + 6:
                                    emit_cast()
                        P.op("pe", gu, reads=[("wgu", wb, q) for q in range(4)] + [("h2T", c, lo // 512) for c in range(NCH)],
                             writes=[("psA", fc), ("psB", fc)])
                        if fc == 0:
                            P.op("pe", lambda E, e=e, lo=lo, n=n: E.matmul(self.psG[:, :n], lhsT=self.selb[:, e:e + 1].broadcast_to([32, 128]),
                                                                            rhs=gT2[0:32, lo:lo + n], start=True, stop=True),
                                 reads=[("selb",), ("gT2", lo // 512)], writes=[("psG",)])
                        P.op("act", lambda E, fc=fc, n=n: E.activation(out=sa[fc][:, :n], in_=self.psA[fc][:, :n], func=AF.Silu),
                             reads=[("psA", fc)], writes=[("m_sa", fc)])
                        if pend is not None:
                            pend(range(4 * fc, 4 * fc + 4))
                        P.op("dve", lambda E, fc=fc, n=n: E.tensor_tensor(out=sa[fc][:, :n], in0=sa[fc][:, :n],
                                                                          in1=self.psB[fc][:, :n], op=ALU.mult),
                             reads=[("m_sa", fc), ("psB", fc)], writes=[("m_sa", fc)])
                        P.op("dve", lambda E, fc=fc, n=n, hb=hb: E.tensor_tensor(out=hid[hb][:, fc, :n], in0=sa[fc][:, :n],
                                                                                 in1=self.psG[:, :n], op=ALU.mult),
                             reads=[("m_sa", fc), ("psG",)], writes=HK[hb])
                    if ti == 0:
                        emit_cast()
                        emit_cast()
                        if nt == 1:
                            for _ in range(4):
                                if e + 1 < NEXP:
                                    emit_cast()

                    def down(dcs, wb=wb, hb=hb, t0=t0, n=n, src=src, g2=g2, stream=stream):
                        for dc in dcs:
                            ob = obank[0] % 3
                            obank[0] += 1
                            po, pok = obanks[ob]

                            def dmm(E, dc=dc, po=po):
                                ins = None
                                for fc in range(2):
                                    ins = E.matmul(po[:, :n], lhsT=wdn[:, fc, dc * 128:(dc + 1) * 128],
                                                   rhs=hid[hb][:, fc, :n], start=(fc == 0), stop=(fc == 1))
                                return ins
                            P.op("pe", dmm, reads=[("wdn", 0), ("wdn", 1)] + HK[hb], writes=[pok])
                            if dc % 2 == 0:
                                P.op("dve", lambda E, dc=dc, po=po: E.scalar_tensor_tensor(
                                    out=src[:, dc, t0:t0 + n], in0=po[:, :n], scalar=g2[:, dc:dc + 1],
                                    in1=src[:, dc, t0:t0 + n], op0=ALU.mult, op1=ALU.add),
                                    reads=[pok] + mk + self.skeys(stream, [dc], t0, n),
                                    writes=self.skeys(stream, [dc], t0, n))
                            else:
                                P.op("act", lambda E, dc=dc, po=po: E.activation(out=rtmp[:, :n], in_=po[:, :n], func=AF.Identity,
                                                                                 scale=g2[:, dc:dc + 1]),
                                     reads=[pok] + mk, writes=[("rt",)])
                                P.op("pool", lambda E, dc=dc: E.tensor_tensor(out=src[:, dc, t0:t0 + n], in0=src[:, dc, t0:t0 + n],
                                                                              in1=rtmp[:, :n], op=ALU.add),
                                     reads=[("rt",)] + self.skeys(stream, [dc], t0, n), writes=self.skeys(stream, [dc], t0, n))
                    pend = down
                    it += 1
            if pend is not None:
                pend(range(NCH))
                pend = None

    def routing(self, l, ns, gT2, lo):
        P = self.P
        rt = self.rt
        o = [0]

        def take(nw, shape):
            a = rt[:, o[0]:o[0] + nw]
            o[0] += nw
            if len(shape) > 1:
                names = " ".join("a%d" % i for i in range(len(shape)))
                kw = {"a%d" % i: s for i, s in enumerate(shape)}
                a = a.rearrange("p (%s) -> p %s" % (names, names), **kw)
            return a
        lgs = take(80, [4, 20])[:, :ns]
        gmax = take(4, [4])[:, :ns]
        gmask = take(16, [4, 4])[:, :ns]
        gd = take(16, [4, 4])[:, :ns]
        gsum = take(4, [4])[:, :ns]
        gw = take(4, [4])[:, :ns]
        em = take(64, [4, 4, 4])[:, :ns]
        ein = take(16, [4, 4])[:, :ns]
        m1 = take(4, [4])[:, :ns]
        mask1 = take(16, [4, 4])[:, :ns]
        e2 = take(16, [4, 4])[:, :ns]
        m2 = take(4, [4])[:, :ns]
        mask2 = take(16, [4, 4])[:, :ns]
        dm = take(4, [4])[:, :ns]
        w1 = take(4, [4])[:, :ns]
        w2 = take(4, [4])[:, :ns]
        ew = take(16, [4, 4])[:, :ns]
        ew2 = take(16, [4, 4])[:, :ns]
        gates = take(64, [4, 16])[:, :ns]
        ghf = take(64, [4, 16])[:, :ns]
        g2t = take(64, [4 * 32]).bitcast(BF16).rearrange("p (s k) -> p s k", k=32)[:, :ns]
        assert o[0] <= 512
        K = ("rt",)
        bc3 = lambda a: a.unsqueeze(2).broadcast_to([128, ns, 4])
        glog = lgs[:, :, 0:4]
        elog = lgs[:, :, 4:20].rearrange("p s (g e) -> p s g e", g=4)

        def dve(fn, extra_r=()):
            P.op("dve", fn, reads=[K] + list(extra_r), writes=[K])

        P.op("dve", lambda E: E.tensor_tensor(out=lgs, in0=self.psG[:, 0:ns * 20].rearrange("p (s k) -> p s k", k=20),
                                              in1=self.br.unsqueeze(1).broadcast_to([128, ns, 20]), op=ALU.add),
             reads=[("psG",), ("br",), K], writes=[K])
        dve(lambda E: E.tensor_reduce(out=gmax, in_=glog, axis=AX.X, op=ALU.max))
        dve(lambda E: E.tensor_tensor(out=gmask, in0=glog, in1=bc3(gmax), op=ALU.is_ge))
        dve(lambda E: E.tensor_tensor(out=gd, in0=glog, in1=bc3(gmax), op=ALU.subtract))
        P.op("act", lambda E: E.activation(out=gd, in_=gd, func=AF.Exp), reads=[K], writes=[K])
        dve(lambda E: E.tensor_reduce(out=gsum, in_=gd, axis=AX.X, op=ALU.add))
        dve(lambda E: E.reciprocal(out=gw, in_=gsum))
        dve(lambda E: E.tensor_tensor(out=em, in0=elog, in1=gmask.unsqueeze(3).broadcast_to([128, ns, 4, 4]), op=ALU.mult))
        dve(lambda E: E.tensor_reduce(out=ein, in_=em.rearrange("p s g e -> p s e g"), axis=AX.X, op=ALU.add))
        dve(lambda E: E.tensor_reduce(out=m1, in_=ein, axis=AX.X, op=ALU.max))
        dve(lambda E: E.tensor_tensor(out=mask1, in0=ein, in1=bc3(m1), op=ALU.is_ge))
        dve(lambda E: E.scalar_tensor_tensor(out=e2, in0=mask1, scalar=-1e30, in1=ein, op0=ALU.mult, op1=ALU.add))
        dve(lambda E: E.tensor_reduce(out=m2, in_=e2, axis=AX.X, op=ALU.max))
        dve(lambda E: E.tensor_tensor(out=mask2, in0=e2, in1=bc3(m2), op=ALU.is_ge))
        dve(lambda E: E.tensor_tensor(out=dm, in0=m2, in1=m1, op=ALU.subtract))
        P.op("act", lambda E: E.activation(out=dm, in_=dm, func=AF.Exp), reads=[K], writes=[K])
        dve(lambda E: E.tensor_scalar(out=w1, in0=dm, scalar1=1.0, scalar2=None, op0=ALU.add))
        dve(lambda E: E.reciprocal(out=w1, in_=w1))
        dve(lambda E: E.tensor_tensor(out=w2, in0=dm, in1=w1, op=ALU.mult))
        dve(lambda E: E.tensor_tensor(out=w1, in0=w1, in1=gw, op=ALU.mult))
        dve(lambda E: E.tensor_tensor(out=w2, in0=w2, in1=gw, op=ALU.mult))
        dve(lambda E: E.tensor_tensor(out=ew, in0=mask1, in1=bc3(w1), op=ALU.mult))
        dve(lambda E: E.tensor_tensor(out=ew2, in0=mask2, in1=bc3(w2), op=ALU.mult))
        dve(lambda E: E.tensor_tensor(out=ew, in0=ew, in1=ew2, op=ALU.add))
        dve(lambda E: E.tensor_tensor(out=gates.rearrange("p s (g e) -> p s g e", g=4),
                                      in0=gmask.unsqueeze(3).broadcast_to([128, ns, 4, 4]),
                                      in1=ew.unsqueeze(2).broadcast_to([128, ns, 4, 4]), op=ALU.mult))
        dve(lambda E: E.tensor_copy(out=g2t[:, :, 0:16], in_=gates))
        dve(lambda E: E.tensor_copy(out=ghf, in_=g2t[:, :, 0:16]))
        dve(lambda E: E.tensor_tensor(out=g2t[:, :, 16:32], in0=gates, in1=ghf, op=ALU.subtract))
        psMb = self.psM.bitcast(BF16)

        def tr(E):
            ins = None
            for s in range(ns):
                ins = E.transpose(out=psMb[0:32, s * 128:(s + 1) * 128], in_=g2t[:, s, :], identity=self.identb)
            return ins
        P.op("pe", tr, reads=[K, ("identb",)], writes=[("psM",)])
        P.op("act", lambda E: E.activation(out=gT2[0:32, lo:lo + ns * 128], in_=psMb[0:32, 0:ns * 128], func=AF.Copy),
             reads=[("psM",)], writes=[("gT2", lo // 512)])

    def pool_mixer(self, l, pj, stream):
        P = self.P
        src, N = self.S(stream)
        j = 0 if stream == "x" else 1
        NHB = min(N, 2048)
        W = NHB + 16
        rstd = self.carve(0, 4096)
        hc = self.carve(4096, 2064)
        sA = self.carve(6160, 2064)
        sB = self.carve(8224, 2064)
        pT = self.carve(10288, 4096, BF16, [2, 4096])
        pwb = self.carve(14384, 1024, BF16, [4, 2, 256])
        sq = [self.carve(15408, 256, BF16), self.carve(15664, 256, BF16)]
        gs1 = self.gs[:, l, 0, j, :]
        sh1 = self.mod(l, 0, j)
        g1 = self.mod(l, 2, j)
        mk = self.modkeys(l)
        cs = self.pcoef[:, 0, :]
        cb = self.pcoef[:, 1, :]
        P.op("pool", lambda E: E.dma_start(out=pwb, in_=self.d_pw[pj]), writes=[("p_w",)], dma=True)
        P.op("dve", lambda E: E.tensor_tensor(out=cs, in0=g1, in1=self.pv[:, pj, 1, :], op=ALU.mult),
             reads=mk + [("pv",)], writes=[("pcoef",)])
        P.op("dve", lambda E: E.tensor_tensor(out=cb, in0=cs, in1=self.pv[:, pj, 0, :], op=ALU.mult),
             reads=[("pcoef",), ("pv",)], writes=[("pcoef",)])
        for t0 in range(0, N, 512):
            n = min(512, N - t0)
            self.rstd_tile(stream, t0, n, rstd[:, t0:t0 + n], ("p_rstd", t0 // 512), sq, [("p_sq0",), ("p_sq1",)])
        for g in range(4):
            w = 2 << g
            for kc in range(2):
                c = 2 * g + kc
                for T0 in range(0, N, NHB):
                    a = max(0, T0 - 8)
                    b = min(N, T0 + NHB + 8)
                    ca, cbb = a - T0 + 8, b - T0 + 8
                    if T0 == 0:
                        P.op("pool", lambda E: E.memset(hc[:, 0:8], 0.0), writes=[("p_hc",)])
                    if T0 + NHB == N:
                        P.op("pool", lambda E: E.memset(hc[:, 8 + NHB:16 + NHB], 0.0), writes=[("p_hc",)])
                    P.op("dve", lambda E, c=c, a=a, b=b, ca=ca, cbb=cbb: E.scalar_tensor_tensor(
                        out=hc[:, ca:cbb], in0=src[:, c, a:b], scalar=gs1[:, c:c + 1], in1=rstd[:, a:b],
                        op0=ALU.mult, op1=ALU.mult),
                        reads=self.skeys(stream, [c], a, b - a) + mk + [("p_rstd", t) for t in _tiles(a, b - a)],
                        writes=[("p_hc",)])
                    P.op("act", lambda E, c=c, ca=ca, cbb=cbb: E.activation(out=hc[:, ca:cbb], in_=hc[:, ca:cbb], func=AF.Identity,
                                                                             bias=sh1[:, c:c + 1], scale=1.0),
                         reads=[("p_hc",)] + mk, writes=[("p_hc",)])
                    bufs = [hc, sA, sB, sA, sB]
                    keys = [("p_hc",), ("p_sA",), ("p_sB",), ("p_sA",), ("p_sB",)]
                    lo_, hi_ = 0, W
                    for lev in range(g + 1):
                        sh = 1 << max(lev - 1, 0)
                        i_, o_ = bufs[lev], bufs[lev + 1]
                        if lev == 0:
                            nlo, nhi = lo_ + 1, hi_
                            P.op("pool", lambda E, i_=i_, o_=o_, nlo=nlo, nhi=nhi: E.tensor_tensor(
                                out=o_[:, nlo:nhi], in0=i_[:, nlo - 1:nhi - 1], in1=i_[:, nlo:nhi], op=ALU.add),
                                reads=[keys[lev]], writes=[keys[lev + 1]])
                        else:
                            nlo, nhi = lo_ + sh, hi_ - sh
                            P.op("pool", lambda E, i_=i_, o_=o_, nlo=nlo, nhi=nhi, sh=sh: E.tensor_tensor(
                                out=o_[:, nlo:nhi], in0=i_[:, nlo - sh:nhi - sh], in1=i_[:, nlo + sh:nhi + sh], op=ALU.add),
                                reads=[keys[lev]], writes=[keys[lev + 1]])
                        lo_, hi_ = nlo, nhi
                    Sb, Sk = bufs[g + 1], keys[g + 1]
                    assert lo_ <= 8 and hi_ >= 8 + NHB
                    eo = 144 + g * 16
                    if T0 == 0:
                        P.op("dve", lambda E, Sb=Sb, eo=eo: E.tensor_tensor(out=Sb[:, 8:16], in0=Sb[:, 8:16],
                                                                            in1=self.consts[:, eo:eo + 8], op=ALU.mult),
                             reads=[Sk, ("consts",)], writes=[Sk])
                    if T0 + NHB == N:
                        P.op("dve", lambda E, Sb=Sb, eo=eo: E.tensor_tensor(out=Sb[:, NHB:NHB + 8], in0=Sb[:, NHB:NHB + 8],
                                                                            in1=self.consts[:, eo + 8:eo + 16], op=ALU.mult),
                             reads=[Sk, ("consts",)], writes=[Sk])
                    P.op("dve", lambda E, Sb=Sb, kc=kc, T0=T0, w=w: E.scalar_tensor_tensor(
                        out=pT[:, kc, T0:T0 + NHB], in0=Sb[:, 8:8 + NHB], scalar=1.0 / w, in1=hc[:, 8:8 + NHB],
                        op0=ALU.mult, op1=ALU.subtract),
                        reads=[Sk, ("p_hc",)], writes=[("p_pT", kc, t) for t in _tiles(T0, NHB)])
            for t0 in range(0, N, 512):
                n = min(512, N - t0)
                for oc in range(2):
                    dc = 2 * g + oc
                    po = self.psO[oc]

                    def pmm(E, g=g, oc=oc, po=po, t0=t0, n=n):
                        ins = None
                        for kc in range(2):
                            ins = E.matmul(po[:, :n], lhsT=pwb[:, g, kc, oc * 128:(oc + 1) * 128], rhs=pT[:, kc, t0:t0 + n],
                                           start=(kc == 0), stop=(kc == 1))
                        return ins
                    P.op("pe", pmm, reads=[("p_w",), ("p_pT", 0, t0 // 512), ("p_pT", 1, t0 // 512)], writes=[("psO", oc)])
                    P.op("dve", lambda E, dc=dc, po=po, t0=t0, n=n: E.scalar_tensor_tensor(
                        out=src[:, dc, t0:t0 + n], in0=po[:, :n], scalar=cs[:, dc:dc + 1], in1=src[:, dc, t0:t0 + n],
                        op0=ALU.mult, op1=ALU.add),
                        reads=[("psO", oc), ("pcoef",)] + self.skeys(stream, [dc], t0, n), writes=self.skeys(stream, [dc], t0, n))
                    P.op("dve", lambda E, dc=dc, t0=t0, n=n: E.tensor_scalar(
                        out=src[:, dc, t0:t0 + n], in0=src[:, dc, t0:t0 + n], scalar1=cb[:, dc:dc + 1], scalar2=None, op0=ALU.add),
                        reads=[("pcoef",)] + self.skeys(stream, [dc], t0, n), writes=self.skeys(stream, [dc], t0, n))

    def headnorm_rope(self, ps_q, n, gvec, rope, tab, out_ap, out_keys, tmp):
        P = self.P
        obk, Rm = self.obk, self.Rm
        sq, rstd, qn, t1, psms, psms_key, psq_key = tmp["sq"], tmp["rstd"], tmp["qn"], tmp["t1"], tmp["psms"], tmp["psms_key"], tmp["psq_key"]
        P.op("act", lambda E: E.activation(out=sq[:, :n], in_=ps_q[:, :n], func=AF.Square),
             reads=[psq_key], writes=[("a_sq",)])
        P.op("pe", lambda E: E.matmul(psms[:, :n], lhsT=obk, rhs=sq[:, :n], start=True, stop=True),
             reads=[("a_sq",), ("a_const",)], writes=[psms_key])
        P.op("act", lambda E: E.activation(out=rstd[:, :n], in_=psms[:, :n], func=AF.Ln, bias=self.epst, scale=1.0),
             reads=[psms_key, ("epst",)], writes=[("a_rstd",)])
        P.op("act", lambda E: E.activation(out=rstd[:, :n], in_=rstd[:, :n], func=AF.Exp, scale=-0.5),
             reads=[("a_rstd",)], writes=[("a_rstd",)])
        outs = out_ap
        if not rope:
            (oa, r0, r1), = outs
            P.op("dve", lambda E: E.scalar_tensor_tensor(out=oa, in0=ps_q[:, :n], scalar=gvec, in1=rstd[:, :n],
                                                         op0=ALU.mult, op1=ALU.mult),
                 reads=[psq_key, ("a_rstd",), ("a_const",)], writes=out_keys)
            return
        P.op("dve", lambda E: E.scalar_tensor_tensor(out=qn[:, :n], in0=ps_q[:, :n], scalar=gvec, in1=rstd[:, :n],
                                                     op0=ALU.mult, op1=ALU.mult),
             reads=[psq_key, ("a_rstd",), ("a_const",)], writes=[("a_qn",)])
        P.op("pe", lambda E: E.matmul(ps_q[:, :n], lhsT=Rm, rhs=qn[:, :n], start=True, stop=True),
             reads=[("a_qn",), ("a_const",)], writes=[psq_key])
        P.op("pool", lambda E: E.tensor_tensor(out=t1[:, :n], in0=qn[:, :n], in1=tab[0][:, :n], op=ALU.mult),
             reads=[("a_qn",), ("a_tab",)], writes=[("a_t1",)])
        P.op("dve", lambda E: E.tensor_tensor(out=rstd[:, :n], in0=ps_q[:, :n], in1=tab[1][:, :n], op=ALU.mult),
             reads=[psq_key, ("a_tab",), ("a_rstd",)], writes=[("a_rstd",)])
        for (oa, r0, r1) in outs:
            P.op("pool", lambda E, oa=oa, r0=r0, r1=r1: E.tensor_tensor(out=oa, in0=t1[r0:r1, :n], in1=rstd[r0:r1, :n], op=ALU.add),
                 reads=[("a_t1",), ("a_rstd",)], writes=out_keys)

    def attn_norm(self, l, stream, t0, n, aT, sq2, rstd, ps=None, ps_key=None, all_pool=False):
        P = self.P
        src, _ = self.S(stream)
        j = 0 if stream == "x" else 1
        gs1 = self.gs[:, l, 0, j, :]
        sh1 = self.mod(l, 0, j)
        mk = self.modkeys(l)
        self.rstd_tile(stream, t0, n, rstd[:, :n], ("a_rstd",), sq2, [("a_sq",), ("a_qn",)], ps=ps, ps_key=ps_key,
                       all_pool=all_pool)
        a_tmp, a_tmpk = list(self.a_tmp), list(self.a_tmpk)
        for c in range(NCH):
            P.op("dve", lambda E, c=c: E.scalar_tensor_tensor(out=a_tmp[c % 2][:, :n], in0=src[:, c, t0:t0 + n],
                                                              scalar=gs1[:, c:c + 1], in1=rstd[:, :n], op0=ALU.mult, op1=ALU.mult),
                 reads=self.skeys(stream, [c], t0, n) + mk + [("a_rstd",)], writes=[a_tmpk[c % 2]])
            P.op("act", lambda E, c=c: E.activation(out=aT[:, c, :n], in_=a_tmp[c % 2][:, :n], func=AF.Identity,
                                                    bias=sh1[:, c:c + 1], scale=1.0),
                 reads=[a_tmpk[c % 2]] + mk, writes=[("a_aT", c)])

    def attn_mixer(self, l):
        P = self.P
        NKT = 34
        KT = self.carve(0, 4352, BF16, [2, 4352])
        V = self.carve(4352, 4352, BF16, [NKT, 256])
        aT = self.carve(8704, 2048, BF16, [NCH, 512])
        tab = [self.carve(10752, 512), self.carve(11264, 512)]
        wq = self.carve(11776, 512, BF16, [NCH, 128])
        wkv = self.carve(11776, 2048, BF16, [NCH, 512])
        QTp = [self.carve(12288, 256, BF16), self.carve(12544, 256, BF16)]
        oT = self.carve(12800, 512, BF16, [2, 512])
        sTf = self.sT.rearrange("p c t -> p (c t)")
        PT = [self.carve(13312, 256, BF16), self.carve(13568, 256, BF16),
              sTf[:, 1024:1280].bitcast(BF16), sTf[:, 1280:1536].bitcast(BF16)]
        sq = self.carve(13824, 256, BF16)
        rstd = self.carve(14080, 512)
        qn = self.carve(14592, 256, BF16)
        t1 = self.carve(14848, 512)
        self.obk = self.carve(15360, 64, BF16)
        self.Rm = self.carve(15424, 64, BF16)
        ones128 = self.carve(15488, 128)
        rec = self.carve(15616, 512)
        ones128b = self.carve(16128, 64, BF16)
        self.a_tmp = [t1, tab[0]]
        self.a_tmpk = [("a_t1",), ("a_tab",)]
        woG = self.sT.rearrange("p c t -> p (c t)")[:, 0:1024].bitcast(BF16).rearrange("p (s d) -> p s d", s=2)
        g1 = self.mod(l, 2, 0)
        mk = self.modkeys(l)
        psQ, psMS = self.psA[0], self.psB[0]
        psS = [self.psA[1], self.psB[1]]
        psOut = [self.psO[0], self.psG]
        psDen = [self.psO[1], self.psM]
        kOut = [("psO", 0), ("psG",)]
        kDen = [("psO", 1), ("psM",)]
        psY = [psQ, psMS]
        kY = [("psA", 0), ("psB", 0)]
        tmp = dict(sq=sq, rstd=rstd, qn=qn, t1=t1, psms=psMS, psms_key=("psB", 0), psq_key=("psA", 0))
        P.op("pool", lambda E: E.memset(self.obk, 0.0), writes=[("a_const",)])
        P.op("pool", lambda E: E.memset(self.obk[0:64, 0:64], 1.0 / 64), writes=[("a_const",)])
        P.op("pool", lambda E: E.memset(self.obk[64:128, 64:128], 1.0 / 64), writes=[("a_const",)])
        idv = self.ident.rearrange("p (b h k) -> p b h k", h=2, k=16)
        rmv = self.Rm.rearrange("p (b h k) -> p b h k", h=2, k=16)
        P.op("dve", lambda E: E.tensor_scalar(out=rmv[:, :, 0, :], in0=idv[:, :, 1, :], scalar1=-1.0, scalar2=None, op0=ALU.mult),
             reads=[("consts",)], writes=[("a_const",)])
        P.op("dve", lambda E: E.tensor_copy(out=rmv[:, :, 1, :], in_=idv[:, :, 0, :]), reads=[("consts",)], writes=[("a_const",)])
        P.op("pool", lambda E: E.memset(ones128, 1.0), writes=[("a_const",)])
        P.op("pool", lambda E: E.memset(ones128b, 1.0), writes=[("a_const",)])
        P.op("sp", lambda E: E.dma_start(out=self.av, in_=self.d_av), writes=[("a_av",)], dma=True)
        P.op("dve", lambda E: E.tensor_scalar(out=self.av[:, 0:1], in0=self.av[:, 0:1], scalar1=0.125, scalar2=None, op0=ALU.mult),
             reads=[("a_av",)], writes=[("a_const",)])
        P.op("pool", lambda E: E.dma_start(out=wkv, in_=self.d_wkv), writes=[("a_wkv",)], dma=True)
        segs = [("s", 0, CTXN, 0, False)] + [("x", t0, 512, CTXN + t0, True) for t0 in range(0, SEQ, 512)]
        for (stream, t0, n, k0, rope) in segs:
            self.attn_norm(l, stream, t0, n, aT, [sq, qn], rstd)
            if rope:
                for i in range(2):
                    P.op("sp", lambda E, i=i, t0=t0, n=n: E.dma_start(out=tab[i][:, :n], in_=self.d_rope[i, :, t0:t0 + n]),
                         writes=[("a_tab",)], dma=True)
            for kc in range(2):
                def kmm(E, kc=kc, n=n):
                    ins = None
                    for k in range(NCH):
                        ins = E.matmul(psQ[:, :n], lhsT=wkv[:, k, kc * 128:(kc + 1) * 128], rhs=aT[:, k, :n],
                                       start=(k == 0), stop=(k == NCH - 1))
                    return ins
                P.op("pe", kmm, reads=[("a_wkv",)] + [("a_aT", c) for c in range(NCH)], writes=[("psA", 0)])
                self.headnorm_rope(psQ, n, self.av[:, 1:2], rope, tab, [(KT[:, kc, k0:k0 + n], 0, 128)],
                                   [("a_KT", kc, kt) for kt in range(k0 // 128, (k0 + n) // 128)], tmp)
            for sub in range(n // 128):
                kt = k0 // 128 + sub

                def vmm(E, sub=sub):
                    ins = None
                    for k in range(NCH):
                        ins = E.matmul(psMS[:, 0:256], lhsT=aT[:, k, sub * 128:(sub + 1) * 128], rhs=wkv[:, k, 256:512],
                                       start=(k == 0), stop=(k == NCH - 1))
                    return ins
                P.op("pe", vmm, reads=[("a_wkv",)] + [("a_aT", c) for c in range(NCH)], writes=[("psB", 0)])
                P.op("act", lambda E, kt=kt: E.activation(out=V[:, kt, :], in_=psMS[:, 0:256], func=AF.Copy),
                     reads=[("psB", 0)], writes=[("a_V", kt)])
        P.barrier()
        QT2 = [QTp, [sTf[:, 1536:1792].bitcast(BF16), sTf[:, 1792:2048].bitcast(BF16)]]
        for par in range(2):
            for half in range(2):
                P.op("pool", lambda E, par=par, half=half: E.memset(QT2[par][half], 0.0), writes=[("a_QT", par, half)])

        def make_q(c):
            par = c % 2
            P.begin_capture()
            P.op("pool", lambda E: E.dma_start(out=wq, in_=self.d_wq[c]), writes=[("a_wq",)], dma=True)

            def qmm(E):
                ins = None
                for k in range(NCH):
                    ins = E.matmul(psQ, lhsT=wq[:, k, :], rhs=aT[:, k, :], start=(k == 0), stop=(k == NCH - 1))
                return ins
            P.op("pe", qmm, reads=[("a_wq",)] + [("a_aT", cc) for cc in range(NCH)], writes=[("psA", 0)])
            self.headnorm_rope(psQ, 512, self.av[:, 0:1], True, tab,
                               [(QT2[par][0][0:64, :], 0, 64), (QT2[par][1][64:128, :], 64, 128)],
                               [("a_QT", par, 0), ("a_QT", par, 1)], tmp)
            return P.end_capture()
        hn = 0
        pending_wo = []
        tail = []
        NQT = SEQ // 512

        def qtile_prep(qt_, deferred_mode):
            t0_ = qt_ * 512
            if deferred_mode:
                P.begin_capture()
                self.attn_norm(l, "x", t0_, 512, aT, [sq, qn], rstd, ps=psMS, ps_key=("psB", 0), all_pool=True)
            else:
                self.attn_norm(l, "x", t0_, 512, aT, [sq, qn], rstd)
            for i in range(2):
                P.op("sp", lambda E, i=i: E.dma_start(out=tab[i], in_=self.d_rope[i, :, t0_:t0_ + 512]),
                     writes=[("a_tab",)], dma=True)
            ops = P.end_capture() if deferred_mode else []
            return ops + make_q(0)
        for o_ in qtile_prep(0, False):
            P.emit(o_)
        for qt in range(NQT):
            t0 = qt * 512
            n = 512
            for c in range(NCH):
                cp, ci = divmod(c, 2)
                kc = c // 4
                par = c % 2
                if c == 0 and qt == 0:
                    P.op("pool", lambda E, cp=cp: E.dma_start(out=woG, in_=self.d_wo[cp]), writes=[("a_wo",)], dma=True)
                if c + 1 < NCH:
                    dq = make_q(c + 1)
                elif qt + 1 < NQT:
                    dq = qtile_prep(qt + 1, True)
                else:
                    dq = []
                deferred = pending_wo + dq
                n_wo = len(pending_wo)
                pending_wo = []
                slot = 0
                for half in range(2):
                    r0, r1 = half * 64, half * 64 + 64
                    po, pd = psOut[hn % 2], psDen[hn % 2]
                    pok, pdk = kOut[hn % 2], kDen[hn % 2]
                    hn += 1
                    qtb, qtk = QT2[par][half], ("a_QT", par, half)
                    pe_all = False

                    def pv(kt, kc=kc, po=po, pd=pd, pok=pok, pdk=pdk, pe_all=pe_all):
                        pt, ptk = PT[kt % 4], ("a_PT", kt % 4)
                        P.op("pe", lambda E: E.matmul(po, lhsT=V[:, kt, kc * 128:(kc + 1) * 128], rhs=pt,
                                                      start=(kt == 0), stop=(kt == NKT - 1)),
                             reads=[("a_V", kt), ptk], writes=[pok])
                        if pe_all or kt % 2 == 1 or kt < 6:
                            P.op("pe", lambda E: E.matmul(pd, lhsT=ones128b, rhs=pt, start=(kt == 0),
                                                          stop=(pe_all and kt == NKT - 1)),
                                 reads=[("a_const",), ptk], writes=[pdk])
                        elif kt == 6:
                            P.op("dve", lambda E: E.tensor_copy(out=rec, in_=pt), reads=[ptk], writes=[("a_rec",)])
                        else:
                            P.op("dve", lambda E: E.tensor_tensor(out=rec, in0=rec, in1=pt, op=ALU.add),
                                 reads=[ptk, ("a_rec",)], writes=[("a_rec",)])
                    for kt in range(NKT):
                        P.op("pe", lambda E, kt=kt, kc=kc, qtb=qtb: E.matmul(
                            psS[kt % 2], lhsT=KT[:, kc, kt * 128:(kt + 1) * 128], rhs=qtb, start=True, stop=True),
                            reads=[("a_KT", kc, kt), qtk], writes=[("psS", kt % 2)])
                        P.op("act", lambda E, kt=kt: E.activation(out=PT[kt % 4], in_=psS[kt % 2], func=AF.Exp),
                             reads=[("psS", kt % 2)], writes=[("a_PT", kt % 4)])
                        if tail and kt >= 1:
                            P.emit(tail.pop(0))
                        if kt > 1:
                            pv(kt - 2)
                        slot += 1
                        wo_left = max(0, len(deferred) - len(dq))
                        if deferred and not tail and (wo_left == 0 or kt >= 10):
                            if wo_left > 0 or slot % 2 == 0 or 2 * len(deferred) > (2 * NKT - slot):
                                P.emit(deferred.pop(0))
                    pv(NKT - 2)
                    pv(NKT - 1)
                    while tail:
                        P.emit(tail.pop(0))
                    if half == 0:
                        while len(deferred) > len(dq):
                            P.emit(deferred.pop(0))
                    P.begin_capture()
                    if not pe_all:
                        P.op("pe", lambda E, pd=pd: E.matmul(pd, lhsT=ones128, rhs=rec, start=False, stop=True),
                             reads=[("a_const",), ("a_rec",)], writes=[pdk])
                    for qq in range(4):
                        P.op("dve", lambda E, pd=pd, r0=r0, r1=r1, qq=qq: E.reciprocal(
                            out=rec[r0:r1, qq * 128:(qq + 1) * 128], in_=pd[r0:r1, qq * 128:(qq + 1) * 128]),
                            reads=[pdk], writes=[("a_rec",)])
                    P.op("dve", lambda E, po=po, ci=ci, r0=r0, r1=r1: E.tensor_tensor(
                        out=oT[r0:r1, ci, :], in0=po[r0:r1, :], in1=rec[r0:r1, :], op=ALU.mult),
                        reads=[pok, ("a_rec",)], writes=[("a_oT", ci)])
                    tail = P.end_capture()
                    if c == NCH - 1 and half == 1 and qt == NQT - 1:
                        while tail:
                            P.emit(tail.pop(0))
                for o_ in deferred:
                    P.emit(o_)
                if ci == 1:
                    P.begin_capture()
                    for dm in range(NCH):
                        py, pyk = psY[dm % 2], kY[dm % 2]

                        def omm(E, dm=dm, py=py):
                            ins = None
                            for cj in range(2):
                                ins = E.matmul(py, lhsT=woG[:, cj, dm * 128:(dm + 1) * 128], rhs=oT[:, cj, :],
                                               start=(cj == 0), stop=(cj == 1))
                            return ins
                        P.op("pe", omm, reads=[("a_wo",), ("a_oT", 0), ("a_oT", 1)], writes=[pyk])
                        P.op("dve", lambda E, dm=dm, py=py, t0=t0: E.scalar_tensor_tensor(
                            out=self.xT[:, dm, t0:t0 + 512], in0=py, scalar=g1[:, dm:dm + 1], in1=self.xT[:, dm, t0:t0 + 512],
                            op0=ALU.mult, op1=ALU.add),
                            reads=[pyk] + mk + self.skeys("x", [dm], t0, 512), writes=self.skeys("x", [dm], t0, 512))
                    if cp + 1 < 4 or qt + 1 < NQT:
                        P.op("pool", lambda E, cp=cp: E.dma_start(out=woG, in_=self.d_wo[(cp + 1) % 4]), writes=[("a_wo",)], dma=True)
                    pending_wo = P.end_capture()
                    if cp == 3 and qt == NQT - 1:
                        for o_ in pending_wo:
                            P.emit(o_)
                        pending_wo = []

    def sgu_mixer(self, l):
        P = self.P
        winv = self.carve(0, 4096, BF16, [NCH, 1024])
        wsT = self.carve(4096, 512, BF16, [8, 128])
        Bg = self.carve(4608, 1024, F32, [8, 128])
        aT = self.carve(5632, 2048, BF16, [NCH, 512])
        uv = self.carve(7680, 2048, BF16, [NCH, 512])
        v = self.carve(9728, 1024)
        vn = self.carve(10752, 512, BF16)
        wu = [self.carve(11264, 512, BF16, [NCH, 128]), self.carve(11776, 512, BF16, [NCH, 128])]
        wo = [self.carve(12288, 512, BF16, [NCH, 128]), self.carve(12800, 512, BF16, [NCH, 128])]
        tmpa = self.carve(13568, 512)
        tmpb = self.carve(14080, 512)
        lbb = self.carve(13568, 512, BF16)
        sq0 = self.carve(14592, 256, BF16)
        sq1 = self.carve(14848, 256, BF16)
        rstd = self.carve(15104, 512)
        binv = self.carve(15616, 512, BF16)
        ones1 = self.carve(16128, 64, BF16)
        st = self.carve(16192, 16)
        sgv = self.carve(16208, 16, F32, [2, NCH])
        tsm = self.carve(16224, 128)
        self.a_tmp = [tmpa, tmpb]
        self.a_tmpk = [("a_t1",), ("a_tab",)]
        g1 = self.mod(l, 2, 0)
        mk = self.modkeys(l)
        P.op("pool", lambda E: E.dma_start(out=winv, in_=self.d_winv), writes=[("g_winv",)], dma=True)
        P.op("pool", lambda E: E.dma_start(out=wsT, in_=self.d_wsT), writes=[("g_ws",)], dma=True)
        P.op("pool", lambda E: E.dma_start(out=lbb, in_=self.d_lnb), writes=[("a_t1",)], dma=True)
        P.op("pool", lambda E: E.dma_start(out=binv[0:1, :], in_=self.d_binv), writes=[("g_binv",)], dma=True)
        P.op("sp", lambda E: E.dma_start(out=Bg, in_=self.d_bsb), writes=[("g_Bg",)], dma=True)
        P.op("sp", lambda E: E.dma_start(out=sgv, in_=self.d_sgv), writes=[("g_sgv",)], dma=True)
        P.op("pool", lambda E: E.memset(ones1, 1.0), writes=[("g_ones",)])
        for g in range(8):
            P.op("pe", lambda E, g=g: E.matmul(self.psA[0][:, 0:128], lhsT=lbb[:, g * 128:(g + 1) * 128], rhs=wsT[:, g, :],
                                               start=True, stop=True),
                 reads=[("a_t1",), ("g_ws",)], writes=[("psA", 0)])
            P.op("dve", lambda E, g=g: E.tensor_tensor(out=Bg[:, g, :], in0=self.psA[0][:, 0:128], in1=Bg[:, g, :], op=ALU.add),
                 reads=[("psA", 0), ("g_Bg",)], writes=[("g_Bg",)])
        P.barrier()
        nw = [0, 0]
        for t0 in range(0, SEQ, 512):
            n = 512
            self.attn_norm(l, "x", t0, n, aT, [sq0, sq1], rstd)
            for g in range(8):
                wb = nw[0] % 2
                nw[0] += 1
                P.op("pool", lambda E, g=g, wb=wb: E.dma_start(out=wu[wb], in_=self.d_winu[g]), writes=[("g_wu", wb)], dma=True)
                pu = self.psA[g % 2]

                def umm(E, wb=wb, pu=pu):
                    ins = None
                    for k in range(NCH):
                        ins = E.matmul(pu, lhsT=wu[wb][:, k, :], rhs=aT[:, k, :], start=(k == 0), stop=(k == NCH - 1))
                    return ins
                P.op("pe", umm, reads=[("g_wu", wb)] + [("a_aT", c) for c in range(NCH)], writes=[("psA", g % 2)])
                P.op("act", lambda E, g=g, pu=pu: E.activation(out=uv[:, g, :], in_=pu, func=AF.Gelu_apprx_tanh,
                                                               bias=sgv[:, 0, g:g + 1], scale=1.0),
                     reads=[("psA", g % 2), ("g_sgv",)], writes=[("g_uv", g)])
            for ch in range(4):
                for fb in range(2):
                    pv = self.psB[fb]

                    def vmm(E, ch=ch, fb=fb, pv=pv):
                        E.matmul(pv, lhsT=ones1[0:1, :], rhs=binv[0:1, fb * 512:(fb + 1) * 512], start=True, stop=False)
                        ins = None
                        for k in range(NCH):
                            ins = E.matmul(pv, lhsT=aT[:, k, ch * 128:(ch + 1) * 128], rhs=winv[:, k, fb * 512:(fb + 1) * 512],
                                           start=False, stop=(k == NCH - 1))
                        return ins
                    P.op("pe", vmm, reads=[("g_winv",), ("g_binv",), ("g_ones",)] + [("a_aT", c) for c in range(NCH)],
                         writes=[("psB", fb)])
                    P.op("act", lambda E, fb=fb, pv=pv: E.activation(out=v[:, fb * 512:(fb + 1) * 512], in_=pv, func=AF.Gelu_apprx_tanh,
                                                                     accum_out=st[:, fb:fb + 1]),
                         reads=[("psB", fb)], writes=[("g_v", fb), ("g_st",)])
                P.op("act", lambda E: E.activation(out=vn, in_=v, func=AF.Square, accum_out=st[:, 2:3]),
                     reads=[("g_v", 0), ("g_v", 1)], writes=[("g_vn",), ("g_st",)])
                K = [("g_st",)]
                P.op("dve", lambda E: E.tensor_tensor(out=st[:, 3:4], in0=st[:, 0:1], in1=st[:, 1:2], op=ALU.add), reads=K, writes=K)
                P.op("dve", lambda E: E.tensor_scalar(out=st[:, 3:4], in0=st[:, 3:4], scalar1=1.0 / 1024, scalar2=None, op0=ALU.mult),
                     reads=K, writes=K)
                P.op("dve", lambda E: E.tensor_tensor(out=st[:, 4:5], in0=st[:, 3:4], in1=st[:, 3:4], op=ALU.mult), reads=K, writes=K)
                P.op("dve", lambda E: E.scalar_tensor_tensor(out=st[:, 5:6], in0=st[:, 2:3], scalar=1.0 / 1024, in1=st[:, 4:5],
                                                             op0=ALU.mult, op1=ALU.subtract), reads=K, writes=K)
                P.op("act", lambda E: E.activation(out=st[:, 6:7], in_=st[:, 5:6], func=AF.Ln, bias=self.epst, scale=1.0),
                     reads=K + [("epst",)], writes=K)
                P.op("act", lambda E: E.activation(out=st[:, 6:7], in_=st[:, 6:7], func=AF.Exp, scale=-0.5), reads=K, writes=K)
                P.op("dve", lambda E: E.scalar_tensor_tensor(out=st[:, 7:8], in0=st[:, 3:4], scalar=-1.0, in1=st[:, 6:7],
                                                             op0=ALU.mult, op1=ALU.mult), reads=K, writes=K)
                P.op("act", lambda E: E.activation(out=vn, in_=v, func=AF.Identity, bias=st[:, 7:8], scale=st[:, 6:7]),
                     reads=K + [("g_v", 0), ("g_v", 1)], writes=[("g_vn",)])
                for gb in range(2):
                    psv = self.psO[gb]

                    def smm(E, gb=gb, psv=psv):
                        ins = None
                        for gi in range(4):
                            g = gb * 4 + gi
                            ins = E.matmul(psv[:, gi * 128:(gi + 1) * 128], lhsT=vn[:, g * 128:(g + 1) * 128], rhs=wsT[:, g, :],
                                           start=True, stop=True)
                        return ins
                    P.op("pe", smm, reads=[("g_vn",), ("g_ws",)], writes=[("psO", gb)])
                    for gi in range(4):
                        g = gb * 4 + gi
                        P.op("dve", lambda E, g=g, gi=gi, psv=psv: E.scalar_tensor_tensor(
                            out=tsm, in0=psv[:, gi * 128:(gi + 1) * 128], scalar=sgv[:, 1, g:g + 1], in1=Bg[:, g, :],
                            op0=ALU.mult, op1=ALU.add),
                            reads=[("psO", gb), ("g_sgv",), ("g_Bg",)], writes=[("g_tsm",)])
                        P.op("dve", lambda E, g=g, ch=ch: E.tensor_tensor(out=uv[:, g, ch * 128:(ch + 1) * 128], in0=tsm,
                                                                          in1=uv[:, g, ch * 128:(ch + 1) * 128], op=ALU.mult),
                             reads=[("g_tsm",), ("g_uv", g)], writes=[("g_uv", g)])
            for dm in range(NCH):
                wb = nw[1] % 2
                nw[1] += 1
                P.op("pool", lambda E, dm=dm, wb=wb: E.dma_start(out=wo[wb], in_=self.d_wout[dm]), writes=[("g_wo", wb)], dma=True)
                py = self.psG if dm % 2 == 0 else self.psM
                pyk = ("psG",) if dm % 2 == 0 else ("psM",)

                def omm(E, wb=wb, py=py):
                    ins = None
                    for k in range(NCH):
                        ins = E.matmul(py, lhsT=wo[wb][:, k, :], rhs=uv[:, k, :], start=(k == 0), stop=(k == NCH - 1))
                    return ins
                P.op("pe", omm, reads=[("g_wo", wb)] + [("g_uv", g) for g in range(8)], writes=[pyk])
                P.op("dve", lambda E, dm=dm, py=py, t0=t0: E.scalar_tensor_tensor(
                    out=self.xT[:, dm, t0:t0 + 512], in0=py, scalar=g1[:, dm:dm + 1], in1=self.xT[:, dm, t0:t0 + 512],
                    op0=ALU.mult, op1=ALU.add),
                    reads=[pyk] + mk + self.skeys("x", [dm], t0, 512), writes=self.skeys("x", [dm], t0, 512))

    def epilogue(self, store_s=False):
        P = self.P
        for c in range(NCH):
            for h in range(2):
                P.op("sp", lambda E, c=c, h=h: E.dma_start(out=self.d_out[c, :, h * 2048:(h + 1) * 2048],
                                                           in_=self.xT[:, c, h * 2048:(h + 1) * 2048]),
                     reads=self.skeys("x", [c], h * 2048, 2048), dma=True, final=True)
            if self.store_s:
                P.op("sp", lambda E, c=c: E.dma_start(out=self.d_sout[c], in_=self.sT[:, c, :]),
                     reads=self.skeys("s", [c], 0, CTXN), dma=True, final=True)

    def mixer(self, li, l):
        kind = l % 3
        if kind == 0:
            self.pool_mixer(li, l // 3, "x")
            self.P.barrier()
            if l < 1:
                self.pool_mixer(li, l // 3, "s")
                self.P.barrier()
        elif kind == 1:
            self.attn_mixer(li)
            self.P.barrier()
        else:
            self.sgu_mixer(li)
            self.P.barrier()

    def build(self):
        self.prologue()
        self.P.barrier()
        for li, l in enumerate(self.layers):
            ph = self.phases
            if ph is None or ("mix", l) in ph:
                self.mixer(li, l)
            if ph is None:
                self.moe(li, "xs" if l < 1 else "x")
                self.P.barrier()
            else:
                if ("moe", l) in ph and ("moe_s", l) in ph:
                    self.moe(li, "xs")
                    self.P.barrier()
                elif ("moe", l) in ph:
                    self.moe(li, "x")
                    self.P.barrier()
                elif ("moe_s", l) in ph:
                    self.moe(li, "s")
                    self.P.barrier()
        self.epilogue()
        self.P.finalize()
        return self.nc


def make_consts():
    c = np.zeros((128, 256), np.float32)
    c[:, 0:128] = np.eye(128, dtype=np.float32)
    for k in range(32):
        c[k, 128 + (k % 16)] = 1.0
    for g in range(4):
        w = 2 << g
        for jj in range(8):
            cl = (jj + w // 2) - max(jj - w // 2, 0)
            cr = min(w // 2, 8 - jj) + w // 2
            c[:, 144 + g * 16 + jj] = w / cl
            c[:, 144 + g * 16 + 8 + jj] = w / cr
    return c


def rope_tables():
    t = np.arange(SEQ)
    inv = 10000.0 ** (-np.arange(0, 32, 2, dtype=np.float64) / 32)
    tab = np.zeros((2, 128, SEQ), np.float64)
    for p in range(128):
        d = p % 64
        pos = (t // 64) if d < 32 else (t % 64)
        ang = pos * np.float64(np.float32(inv[d % 16]))
        ang = np.float32(pos.astype(np.float32) * np.float32(inv[d % 16])).astype(np.float64)
        tab[0, p] = np.cos(ang)
        tab[1, p] = np.sin(ang)
    return tab.astype(np.float32)


def fm(v):
    v = np.asarray(v)
    lead = v.shape[:-1]
    return np.ascontiguousarray(np.moveaxis(v.reshape(lead + (NCH, 128)), -1, 0))


def prep_shared(inp, layers):
    L = len(layers)
    sh = {}
    sh["consts"] = make_consts()
    wm = inp["w_mod"][layers]
    sh["wmod"] = np.ascontiguousarray(wm.reshape(L, NCH, 128, 12, 512).transpose(0, 3, 2, 1, 4))
    sh["bmod"] = np.ascontiguousarray(inp["b_mod"][layers].reshape(L, 48, 128).transpose(2, 0, 1))
    ng = np.stack([inp["norm1_g"][layers], inp["norm2_g"][layers]], axis=1)
    sh["ng"] = fm(ng)
    wr = np.concatenate([inp["moe_wrg"][layers], inp["moe_wre"][layers]], axis=-1)
    sh["wr"] = np.ascontiguousarray(wr.reshape(L, NCH, 128, 20).transpose(2, 0, 1, 3))
    br = np.concatenate([inp["moe_brg"][layers], inp["moe_bre"][layers]], axis=-1)
    sh["br"] = np.ascontiguousarray(np.broadcast_to(br[None], (128, L, 20)))
    wgu = np.concatenate([inp["moe_wg"][layers], inp["moe_wu"][layers]], axis=-1)
    sh["wgu"] = np.ascontiguousarray(wgu.reshape(L, NEXP, NCH, 128, 512).transpose(0, 1, 3, 2, 4))
    wd = inp["moe_wd"][layers]
    sh["wd"] = np.ascontiguousarray(wd.reshape(L, NEXP, 2, 128, 1024).transpose(0, 1, 3, 2, 4))
    pw = inp["pool_w"]
    sh["pw"] = np.ascontiguousarray(pw.reshape(2, 4, 2, 128, 256).transpose(0, 3, 1, 2, 4))
    pv = np.stack([inp["pool_b"].reshape(2, D), inp["pool_ls"]], axis=1)
    sh["pv"] = fm(pv)
    heads = [(c, 4 + c) if c < 4 else (8 + c - 4, 12 + c - 4) for c in range(NCH)]
    wqkv = inp["attn_wqkv"][0]
    wqh = wqkv[:, :1024].reshape(D, 16, 64)
    wq = np.stack([np.concatenate([wqh[:, a], wqh[:, b]], axis=-1) for a, b in heads], axis=0)
    sh["wq"] = np.ascontiguousarray(wq.reshape(NCH, NCH, 128, 128).transpose(0, 2, 1, 3))
    sh["wkv"] = np.ascontiguousarray(wqkv[:, 1024:].reshape(NCH, 128, 512).transpose(1, 0, 2))
    woh = inp["attn_wo"][0].reshape(16, 64, D)
    order = [h for ab in heads for h in ab]
    sh["wo"] = np.ascontiguousarray(woh[order].reshape(4, 2, 128, D).transpose(0, 2, 1, 3))
    win = inp["sgu_win"][0]
    sh["winu"] = np.ascontiguousarray(win[:, :1024].reshape(NCH, 128, NCH, 128).transpose(2, 1, 0, 3))
    sh["winv"] = np.ascontiguousarray(win[:, 1024:].reshape(NCH, 128, 1024).transpose(1, 0, 2))
    sh["wout"] = np.ascontiguousarray(inp["sgu_wout"][0].reshape(NCH, 128, NCH, 128).transpose(2, 1, 0, 3))
    sh["sgv"] = fm(np.stack([inp["sgu_bin"][0][:1024], inp["sgu_lng"][0]], axis=0))
    sh["binv"] = np.ascontiguousarray(inp["sgu_bin"][0][1024:][None])
    sh["lnb"] = np.ascontiguousarray(np.broadcast_to(inp["sgu_lnb"][0][None], (128, 1024)))
    sh["bsb"] = np.ascontiguousarray(np.broadcast_to(inp["sgu_bs"][0][None], (128, 8, 128)))
    sh["wsT"] = np.ascontiguousarray(inp["sgu_ws"][0].transpose(2, 0, 1))
    sh["rope"] = rope_tables()
    sh["av"] = np.ascontiguousarray(np.stack([np.tile(inp["attn_qg"][0], 2), np.tile(inp["attn_kg"][0], 2)], axis=-1))
    return sh


def prep_core(inp, b):
    pc = {}
    pc["xT"] = np.ascontiguousarray(inp["x"][b].T.reshape(NCH, 128, SEQ))
    pc["sT"] = np.ascontiguousarray(inp["ctx"][b].T.reshape(NCH, 128, CTXN))
    cc = np.stack([inp["c"][b], inp["c_ctx"]], axis=-1)
    pc["ccT"] = np.ascontiguousarray(cc.reshape(NCH, 128, 2).transpose(1, 0, 2))
    return pc


def kernel(**inputs):
    inp = {k: np.asarray(v) for k, v in inputs.items()}
    layers = list(range(DEPTH))
    nc = bass.Bass("TRN2", target_bir_lowering=False)
    Builder(nc, layers, store_s=False).build()
    sh = prep_shared(inp, layers)
    in_maps = []
    for b in range(8):
        m = dict(sh)
        m.update(prep_core(inp, b))
        in_maps.append(m)
    res = run_bass_kernel_spmd(nc, in_maps, core_ids=list(range(8)))
    out = np.stack([r["yT"].reshape(D, SEQ).T for r in res.results], axis=0)
    return np.ascontiguousarray(out.astype(np.float32))
```

```python
import numpy as np
import concourse.bass as bass
import concourse.mybir as mybir
from concourse.bass_utils import run_bass_kernel_spmd

F32, BF16 = mybir.dt.float32, mybir.dt.bfloat16
AF = mybir.ActivationFunctionType
ALU = mybir.AluOpType
AX = mybir.AxisListType

D = 1024
SEQ = 4096
CTXN = 256
DEPTH = 4
NCH = 8
EPS = 1e-6
NEXP = 16


class Prog:
    NDSEM = 12

    def __init__(self, nc):
        self.nc = nc
        self.ops = []
        self.eng = {"pe": nc.tensor, "act": nc.scalar, "dve": nc.vector, "pool": nc.gpsimd, "sp": nc.sync}

    def op(self, eng, fn, reads=(), writes=(), dma=False, final=False):
        o = dict(eng=eng, fn=fn, reads=tuple(reads), writes=tuple(writes), dma=dma, final=final, barrier=False)
        if getattr(self, "_cap", None) is not None:
            self._cap.append(o)
        else:
            self.ops.append(o)

    def begin_capture(self):
        self._cap = []

    def end_capture(self):
        c, self._cap = self._cap, None
        return c

    def emit(self, o):
        self.ops.append(o)

    def barrier(self):
        self.ops.append(dict(barrier=True))

    def finalize(self):
        nc = self.nc
        ops = []
        last_on = {}
        dmas = []
        pending = {}
        for o in self.ops:
            if o["barrier"]:
                bd = set(last_on.values()) | set(dmas)
                dmas = []
                for e in self.eng:
                    pending[e] = pending.get(e, set()) | bd
                continue
            idx = len(ops)
            o["bdeps"] = pending.pop(o["eng"], set())
            if o["dma"]:
                dmas.append(idx)
            else:
                last_on[o["eng"]] = idx
            ops.append(o)
        last_w, readers = {}, {}
        for idx, o in enumerate(ops):
            deps = set(o["bdeps"])
            for k in o["reads"]:
                if k in last_w:
                    deps.add(last_w[k])
            for k in o["writes"]:
                if k in last_w:
                    deps.add(last_w[k])
                deps.update(readers.get(k, ()))
            deps.discard(idx)
            if o["eng"] == "pe" and not o["dma"]:
                deps = {d for d in deps if not (ops[d]["eng"] == "pe" and not ops[d]["dma"])}
            o["deps"] = deps
            for k in o["reads"]:
                readers.setdefault(k, []).append(idx)
            for k in o["writes"]:
                last_w[k] = idx
                readers[k] = []
        for o in ops:
            o["signal"] = False
        for o in ops:
            for d in o["deps"]:
                ops[d]["signal"] = True
        csem = {e: nc.alloc_semaphore("cs_" + e) for e in ("pe", "act", "dve", "pool")}
        dsem = {q: [nc.alloc_semaphore("ds_%s%d" % (q, j)) for j in range(self.NDSEM)] for q in ("sp", "pool", "act")}
        cnt = {e: 0 for e in csem}
        dcnt = {q: 0 for q in dsem}
        for o in ops:
            if o["dma"]:
                q = o["eng"]
                j = dcnt[q]
                dcnt[q] += 1
                o["dsem"] = dsem[q][j % self.NDSEM]
                o["dval"] = 16 * (j // self.NDSEM + 1)
                o["dprev"] = 16 * (j // self.NDSEM)
            elif o["signal"]:
                cnt[o["eng"]] += 1
                o["sval"] = cnt[o["eng"]]
        waited = {e: {} for e in self.eng}
        finals = []
        for o in ops:
            e = o["eng"]
            E = self.eng[e]
            need = {}
            for d in o["deps"]:
                p = ops[d]
                if p["dma"]:
                    s, v = p["dsem"], p["dval"]
                else:
                    s, v = csem[p["eng"]], p["sval"]
                if need.get(s, 0) < v:
                    need[s] = v
            if o["dma"] and o["dprev"] > 0:
                s = o["dsem"]
                if need.get(s, 0) < o["dprev"]:
                    need[s] = o["dprev"]
            for s, v in need.items():
                if waited[e].get(s, 0) < v:
                    E.wait_ge(s, v)
                    waited[e][s] = v
            ins = o["fn"](E)
            if o["dma"]:
                ins.then_inc(o["dsem"], 16)
                if o["final"]:
                    finals.append((o["dsem"], o["dval"]))
            elif o["signal"]:
                ins.then_inc(csem[e], 1)
        for s, v in finals:
            nc.sync.wait_ge(s, v)


def _keys(name, *ranges):
    out = [(name,)]
    for r in ranges:
        out = [k + (v,) for k in out for v in r]
    return out


def _tiles(t0, n, step=512):
    return range(t0 // step, (t0 + n - 1) // step + 1)


class Builder:
    def __init__(self, nc, layers, phases=None, store_s=True):
        self.nc = nc
        self.store_s = store_s
        self.P = Prog(nc)
        self.layers = list(layers)
        self.phases = phases
        L = len(self.layers)
        self.L = L
        dr = lambda name, shape: nc.dram_tensor(name, list(shape), F32, kind="ExternalInput").ap()
        self.d_xT = dr("xT", [NCH, 128, SEQ])
        self.d_sT = dr("sT", [NCH, 128, CTXN])
        self.d_cc = dr("ccT", [128, NCH, 2])
        self.d_consts = dr("consts", [128, 256])
        self.d_wmod = dr("wmod", [L, 12, 128, NCH, 512])
        self.d_bmod = dr("bmod", [128, L, 48])
        self.d_ng = dr("ng", [128, L, 2, NCH])
        self.d_wr = dr("wr", [128, L, NCH, 20])
        self.d_br = dr("br", [128, L, 20])
        self.d_wgu = dr("wgu", [L, NEXP, 128, NCH, 512])
        self.d_wd = dr("wd", [L, NEXP, 128, 2, 1024])
        self.d_pw = dr("pw", [2, 128, 4, 2, 256])
        self.d_pv = dr("pv", [128, 2, 2, NCH])
        self.d_wq = dr("wq", [NCH, 128, NCH, 128])
        self.d_wkv = dr("wkv", [128, NCH, 512])
        self.d_wo = dr("wo", [4, 128, 2, 1024])
        self.d_rope = dr("rope", [2, 128, SEQ])
        self.d_av = dr("av", [128, 2])
        self.d_winu = dr("winu", [NCH, 128, NCH, 128])
        self.d_winv = dr("winv", [128, NCH, 1024])
        self.d_wout = dr("wout", [NCH, 128, NCH, 128])
        self.d_sgv = dr("sgv", [128, 2, NCH])
        self.d_binv = dr("binv", [1, 1024])
        self.d_lnb = dr("lnb", [128, 1024])
        self.d_bsb = dr("bsb", [128, 8, 128])
        self.d_wsT = dr("wsT", [128, 8, 128])
        self.d_out = nc.dram_tensor("yT", [NCH, 128, SEQ], F32, kind="ExternalOutput").ap()
        if store_s:
            self.d_sout = nc.dram_tensor("soT", [NCH, 128, CTXN], F32, kind="ExternalOutput").ap()
        self.alloc()

    def alloc(self):
        nc = self.nc
        L = self.L
        sb = lambda name, shape, dt=F32: nc.alloc_sbuf_tensor(name, list(shape), dt).ap()
        self.xT = sb("xT_sb", [128, NCH, SEQ])
        self.sT = sb("sT_sb", [128, NCH, CTXN])
        self.consts = sb("consts_sb", [128, 256])
        self.ident = self.consts[:, 0:128]
        self.identb = sb("identb", [128, 128], BF16)
        self.onesb = sb("onesb", [128, 128], BF16)
        self.selb = sb("selb", [32, NEXP], BF16)
        self.epst = sb("epst", [128, 1])
        self.ccT = sb("ccT_sb", [128, NCH, 2])
        self.scT = sb("scT", [128, NCH, 2])
        self.mods = sb("mods", [128, L, 48, 2])
        self.ng = sb("ng_sb", [128, L, 2, NCH])
        self.gs = sb("gs", [128, L, 2, 2, NCH])
        self.wr = sb("wr_sb", [128, NCH, 20])
        self.br = sb("br_sb", [128, 20])
        self.wrb = sb("wrb", [128, 2, NCH, 20], BF16)
        self.pv = sb("pv_sb", [128, 2, 2, NCH])
        self.pcoef = sb("pcoef", [128, 2, NCH])
        self.rt = sb("rt", [128, 512])
        self.av = sb("av_sb", [128, 2])
        ARENA = 16384
        self.arena = sb("arena", [128, ARENA])
        self.ARENA = ARENA
        ps = lambda name: nc.alloc_psum_tensor(name, [128, 512], F32).ap()
        self.psA = [ps("psA0"), ps("psA1")]
        self.psB = [ps("psB0"), ps("psB1")]
        self.psG = ps("psG")
        self.psO = [ps("psO0"), ps("psO1")]
        self.psM = ps("psM")

    def dbg(self, name, ap, keys):
        if not getattr(self, "debug", False):
            return
        shp = list(ap.shape)
        d = self.nc.dram_tensor("dbg_" + name, shp, F32, kind="ExternalOutput").ap()
        self.P.op("pool", lambda E: E.dma_start(out=d, in_=ap), reads=keys, dma=True, final=True)

    def carve(self, off, nwords, dt=F32, shape=None):
        assert off + nwords <= self.ARENA, (off, nwords)
        a = self.arena[:, off:off + nwords]
        if dt == BF16:
            a = a.bitcast(BF16)
        if shape is not None:
            names = " ".join("a%d" % i for i in range(len(shape)))
            kw = {"a%d" % i: s for i, s in enumerate(shape)}
            a = a.rearrange("p (%s) -> p %s" % (names, names), **kw)
        return a

    def S(self, stream):
        return (self.xT, SEQ) if stream == "x" else (self.sT, CTXN)

    def skeys(self, stream, chunks, t0, n):
        return _keys(stream, chunks, _tiles(t0, n))

    def prologue(self):
        P, nc = self.P, self.nc
        L = self.L
        self.bmod = self.carve(8192, L * 48, F32, [L, 48])
        P.op("sp", lambda E: E.dma_start(out=self.consts, in_=self.d_consts), writes=[("consts",)], dma=True)
        P.op("sp", lambda E: E.dma_start(out=self.ccT, in_=self.d_cc), writes=[("ccT",)], dma=True)
        P.op("sp", lambda E: E.dma_start(out=self.bmod, in_=self.d_bmod), writes=[("bmod",)], dma=True)
        P.op("sp", lambda E: E.dma_start(out=self.ng, in_=self.d_ng), writes=[("ng",)], dma=True)
        P.op("sp", lambda E: E.dma_start(out=self.pv, in_=self.d_pv), writes=[("pv",)], dma=True)
        for c in range(NCH):
            for h in range(2):
                P.op("sp", lambda E, c=c, h=h: E.dma_start(out=self.xT[:, c, h * 2048:(h + 1) * 2048],
                                                           in_=self.d_xT[c, :, h * 2048:(h + 1) * 2048]),
                     writes=self.skeys("x", [c], h * 2048, 2048), dma=True)
            P.op("sp", lambda E, c=c: E.dma_start(out=self.sT[:, c, :], in_=self.d_sT[c]),
                 writes=self.skeys("s", [c], 0, CTXN), dma=True)
        P.op("dve", lambda E: E.tensor_copy(out=self.identb, in_=self.ident), reads=[("consts",)], writes=[("identb",)])
        P.op("pool", lambda E: E.memset(self.onesb, 1.0 / D), writes=[("onesb",)])
        P.op("pool", lambda E: E.memset(self.epst, EPS), writes=[("epst",)])
        P.op("dve", lambda E: E.tensor_copy(out=self.selb, in_=self.consts[0:32, 128:144]),
             reads=[("consts",)], writes=[("selb",)])
        scTb = self.carve(12544, 8, BF16, [NCH, 2])
        P.op("act", lambda E: E.activation(out=scTb, in_=self.ccT, func=AF.Silu), reads=[("ccT",)], writes=[("scT",)])
        wm = [self.carve(0, 4096, F32, [NCH, 512]), self.carve(4096, 4096, F32, [NCH, 512])]
        wmb = [self.carve(8448, 2048, BF16, [NCH, 512]), self.carve(10496, 2048, BF16, [NCH, 512])]
        n = 0
        for l in range(L):
            for blk in range(12):
                buf = wm[n % 2]
                bufb = wmb[n % 2]
                for kh in range(2):
                    P.op("sp", lambda E, l=l, blk=blk, buf=buf, kh=kh: E.dma_start(out=buf[:, kh * 4:(kh + 1) * 4, :],
                                                                                    in_=self.d_wmod[l, blk, :, kh * 4:(kh + 1) * 4, :]),
                         writes=[("wm", n % 2, kh)], dma=True)
                    if kh == 0:
                        P.op("act", lambda E, buf=buf, bufb=bufb: E.activation(out=bufb[:, 0:4, :], in_=buf[:, 0:4, :], func=AF.Copy),
                             reads=[("wm", n % 2, 0)], writes=[("wmb", n % 2, 0)])
                    else:
                        P.op("dve", lambda E, buf=buf, bufb=bufb: E.tensor_copy(out=bufb[:, 4:8, :], in_=buf[:, 4:8, :]),
                             reads=[("wm", n % 2, 1)], writes=[("wmb", n % 2, 1)])

                def mm(E, bufb=bufb):
                    ins = None
                    for f in range(4):
                        for k in range(NCH):
                            ins = E.matmul(self.psM[:, 2 * f:2 * f + 2], lhsT=bufb[:, k, f * 128:(f + 1) * 128],
                                           rhs=scTb[:, k, :], start=(k == 0), stop=(k == NCH - 1))
                    return ins
                P.op("pe", mm, reads=[("wmb", n % 2, 0), ("wmb", n % 2, 1), ("scT",)], writes=[("psM",)])
                P.op("dve", lambda E, l=l, blk=blk: E.tensor_tensor(
                    out=self.mods[:, l, blk * 4:(blk + 1) * 4, :],
                    in0=self.psM[:, 0:8].rearrange("p (f j) -> p f j", j=2),
                    in1=self.bmod[:, l, blk * 4:(blk + 1) * 4].unsqueeze(2).broadcast_to([128, 4, 2]), op=ALU.add),
                    reads=[("psM",), ("bmod",)], writes=[("mods", l, blk)])
                n += 1
            for nn in range(2):
                for j in range(2):
                    q0 = 8 + 24 * nn
                    P.op("dve", lambda E, l=l, nn=nn, j=j, q0=q0: E.scalar_tensor_tensor(
                        out=self.gs[:, l, nn, j, :], in0=self.mods[:, l, q0:q0 + 8, j], scalar=1.0,
                        in1=self.ng[:, l, nn, :], op0=ALU.add, op1=ALU.mult),
                        reads=[("mods", l, b) for b in range(12)] + [("ng",)], writes=[("gs", l)])
        self.wm_keys = [("wm", 0), ("wm", 1)]

    def mod(self, l, s, j):
        return self.mods[:, l, s * 8:(s + 1) * 8, j]

    def modkeys(self, l):
        return [("mods", l, b) for b in range(12)] + [("gs", l)]

    def rstd_tile(self, stream, t0, n, out_ap, out_key, sq, sqkeys, ps=None, ps_key=None, all_pool=False):
        P = self.P
        src, _ = self.S(stream)
        if ps is None:
            ps, ps_key = self.psM, ("psM",)
        for c in range(NCH):
            if c % 2 == 0 or all_pool:
                P.op("pool", lambda E, c=c: E.tensor_tensor(out=sq[c % 2][:, :n], in0=src[:, c, t0:t0 + n],
                                                             in1=src[:, c, t0:t0 + n], op=ALU.mult),
                     reads=self.skeys(stream, [c], t0, n), writes=[sqkeys[c % 2]])
            else:
                P.op("act", lambda E, c=c: E.activation(out=sq[c % 2][:, :n], in_=src[:, c, t0:t0 + n], func=AF.Square),
                     reads=self.skeys(stream, [c], t0, n), writes=[sqkeys[c % 2]])
            P.op("pe", lambda E, c=c: E.matmul(ps[:, :n], lhsT=self.onesb, rhs=sq[c % 2][:, :n],
                                               start=(c == 0), stop=(c == NCH - 1)),
                 reads=[sqkeys[c % 2], ("onesb",)], writes=[ps_key])
        P.op("act", lambda E: E.activation(out=out_ap, in_=ps[:, :n], func=AF.Ln, bias=self.epst, scale=1.0),
             reads=[ps_key, ("epst",)], writes=[out_key])
        P.op("act", lambda E: E.activation(out=out_ap, in_=out_ap, func=AF.Exp, scale=-0.5),
             reads=[out_key], writes=[out_key])

    def moe(self, l, streams):
        P = self.P
        big = (streams == "x" and self.layers[l] >= 1)
        NH = 2048 if big else 1536
        passes = []
        if "x" in streams:
            for T0 in range(0, SEQ, NH):
                NP = min(NH, SEQ - T0)
                passes.append([("x", T0 + k, min(512, NP - k), k) for k in range(0, NP, 512)])
            if "s" in streams:
                passes[-1].append(("s", 0, CTXN, 1024))
        else:
            passes.append([("s", 0, CTXN, 0)])

        def sv(stream):
            j = 0 if stream == "x" else 1
            return self.S(stream)[0], self.gs[:, l, 1, j, :], self.mod(l, 3, j), self.mod(l, 5, j)
        if big:
            h2T = self.carve(0, 8192, BF16, [NCH, 2048])
            sTf = self.sT.rearrange("p c t -> p (c t)")
            stg = [sTf[:, 0:1024], sTf[:, 1024:2048]]
        else:
            h2T = self.carve(0, 6144, BF16, [NCH, 1536])
            stg = [self.carve(6144, 1024), self.carve(7168, 1024)]
        wgu = [self.carve(8192, 2048, BF16, [NCH, 512]), self.carve(10240, 2048, BF16, [NCH, 512])]
        wdn = self.carve(12288, 1024, BF16, [2, 1024])
        gT2 = self.carve(13312, 1024, BF16, [2048])
        tmpf = [self.carve(14336, 512), self.carve(14848, 512)]
        sq = [self.carve(15360, 256, BF16), self.carve(15616, 256, BF16)]
        rstd = self.carve(15872, 512)
        sa = tmpf
        hid = [self.carve(15360, 512, BF16, [2, 512]), self.carve(15872, 512, BF16, [2, 512])]
        HK = [[("m_h0a",), ("m_h0b",)], [("m_h1",)]]
        mk = self.modkeys(l)
        rt = self.rt
        P.op("sp", lambda E: E.dma_start(out=self.wr, in_=self.d_wr[:, l]), writes=[("wr",)], dma=True)
        P.op("sp", lambda E: E.dma_start(out=self.br, in_=self.d_br[:, l]), writes=[("br",)], dma=True)
        wrf = self.wr.rearrange("p c k -> p (c k)")
        wtmp = self.rt[:, 0:160]
        P.op("dve", lambda E: E.tensor_copy(out=self.wrb[:, 0], in_=self.wr), reads=[("wr",)], writes=[("wrb",)])
        P.op("dve", lambda E: E.tensor_copy(out=wtmp, in_=self.wrb[:, 0].rearrange("p c k -> p (c k)")),
             reads=[("wrb",)], writes=[("rt",)])
        P.op("dve", lambda E: E.tensor_tensor(out=wtmp, in0=wrf, in1=wtmp, op=ALU.subtract),
             reads=[("wr",), ("rt",)], writes=[("rt",)])
        P.op("dve", lambda E: E.tensor_copy(out=self.wrb[:, 1].rearrange("p c k -> p (c k)"), in_=wtmp),
             reads=[("rt",)], writes=[("wrb",)])
        for pi, tiles in enumerate(passes):
            nt = len(tiles)
            seq = []

            def wgu_chunks(e):
                return [(self.d_wgu[l, e, :, 2 * q:2 * q + 2, :], wgu[e % 2][:, 2 * q:2 * q + 2, :], ("wgu", e % 2, q), True)
                        for q in range(4)]

            def wd_chunks(e):
                return [(self.d_wd[l, e, :, q, :], wdn[:, q, :], ("wdn", q), False) for q in range(2)]
            seq += wgu_chunks(0)
            for e in range(NEXP):
                seq += wd_chunks(e)
                if e + 1 < NEXP:
                    seq += wgu_chunks(e + 1)
            cnt = dict(d=0, c=0)

            def issue_dma():
                i = cnt["d"]
                if i >= len(seq):
                    return
                cnt["d"] += 1
                srcap, _, _, two = seq[i]
                sb_ = stg[i % 2]
                dst = sb_.rearrange("p (a b) -> p a b", a=2) if two else sb_
                P.op("sp", lambda E: E.dma_start(out=dst, in_=srcap), writes=[("stg", i % 2)], dma=True)

            def emit_cast():
                i = cnt["c"]
                if i >= len(seq):
                    return
                cnt["c"] += 1
                _, dstap, dkey, two = seq[i]
                sb_ = stg[i % 2]
                sv = sb_.rearrange("p (a b) -> p a b", a=2) if two else sb_
                P.op("act", lambda E: E.activation(out=dstap, in_=sv, func=AF.Copy), reads=[("stg", i % 2)], writes=[dkey])
                issue_dma()
            issue_dma()
            issue_dma()
            def emit_rstd(tile):
                stream_, t0_, n_, _ = tile
                self.rstd_tile(stream_, t0_, n_, rstd[:, :n_], ("m_h1",), sq, [("m_h0a",), ("m_h0b",)])
            emit_rstd(tiles[0])
            for tidx, (stream, t0, n, lo) in enumerate(tiles):
                src, gs2, sh2, g2 = sv(stream)
                ns = n // 128
                for c in range(NCH):
                    tb = tmpf[c % 2]
                    P.op("dve", lambda E, c=c, tb=tb, t0=t0, n=n, src=src, gs2=gs2: E.scalar_tensor_tensor(
                        out=tb[:, :n], in0=src[:, c, t0:t0 + n], scalar=gs2[:, c:c + 1], in1=rstd[:, :n],
                        op0=ALU.mult, op1=ALU.mult),
                        reads=self.skeys(stream, [c], t0, n) + mk + [("m_h1",)], writes=[("m_sa", c % 2)])
                    P.op("act", lambda E, c=c, tb=tb, lo=lo, n=n, sh2=sh2: E.activation(out=h2T[:, c, lo:lo + n], in_=tb[:, :n], func=AF.Identity,
                                                                   bias=sh2[:, c:c + 1], scale=1.0),
                         reads=[("m_sa", c % 2)] + mk, writes=[("h2T", c, lo // 512)])

                def rmm(E, lo=lo, ns=ns):
                    ins = None
                    for s in range(ns):
                        for c in range(NCH):
                            for part in range(2):
                                ins = E.matmul(self.psG[:, s * 20:(s + 1) * 20],
                                               lhsT=h2T[:, c, lo + s * 128:lo + (s + 1) * 128],
                                               rhs=self.wrb[:, part, c, :], start=(c == 0 and part == 0),
                                               stop=(c == NCH - 1 and part == 1))
                    return ins
                P.op("pe", rmm, reads=[("h2T", c, lo // 512) for c in range(NCH)] + [("wrb",)], writes=[("psG",)])
                if tidx == 0:
                    for _ in range(4):
                        emit_cast()
                if tidx + 1 < nt:
                    emit_rstd(tiles[tidx + 1])
                self.routing(l, ns, gT2, lo)
                if lo == 0 and pi == 0:
                    self.dbg("lgs_" + stream, self.rt[:, 0:80], [("rt",)])
                    self.dbg("rt_" + stream, self.rt[:, 0:512], [("rt",)])
                    self.dbg("gT2_" + stream, gT2[0:32, 0:n], [("gT2", 0)])
                    self.dbg("h2T_" + stream, h2T[:, 0, 0:n], [("h2T", 0, 0)])
                    self.dbg("h2T7_" + stream, h2T[:, 7, 0:n], [("h2T", 7, 0)])
                    self.dbg("tmp0_" + stream, tmpf[0][:, :n], [("m_sa", 0)])
                    self.dbg("rstd_" + stream, rstd[:, :n], [("m_h1",)])
                    self.dbg("xin_" + stream, src[:, 0, 0:n], self.skeys(stream, [0], 0, n))
            obanks = [(self.psO[0], ("psO", 0)), (self.psO[1], ("psO", 1)), (self.psM, ("psM",))]
            obank = [0]
            rtmp = self.rt
            pend = None
            it = 0
            for e in range(NEXP):
                wb = e % 2
                for ti, (stream, t0, n, lo) in enumerate(tiles):
                    src, gs2, sh2, g2 = sv(stream)
                    hb = it % 2
                    for fc in range(2):
                        def gu(E, fc=fc, wb=wb, lo=lo, n=n):
                            ins = None
                            for half, pst in ((0, self.psA[fc]), (1, self.psB[fc])):
                                for k in range(NCH):
                                    col = half * 256 + fc * 128
                                    ins = E.matmul(pst[:, :n], lhsT=wgu[wb][:, k, col:col + 128],
                                                   rhs=h2T[:, k, lo:lo + n], start=(k == 0), stop=(k == NCH - 1))
                            return ins
                        if ti >= 1 and e + 1 < NEXP:
                            per = -(-4 // (2 * (nt - 1)))
                            for _ in range(per):
                                if cnt["c"] < 4 + 6 * e + 6:
                                    emit_cast()
                        P.op("pe", gu, reads=[("wgu", wb, q) for q in range(4)] + [("h2T", c, lo // 512) for c in range(NCH)],
                             writes=[("psA", fc), ("psB", fc)])
                        if fc == 0:
                            P.op("pe", lambda E, e=e, lo=lo, n=n: E.matmul(self.psG[:, :n], lhsT=self.selb[:, e:e + 1].broadcast_to([32, 128]),
                                                                            rhs=gT2[0:32, lo:lo + n], start=True, stop=True),
                                 reads=[("selb",), ("gT2", lo // 512)], writes=[("psG",)])
                        P.op("act", lambda E, fc=fc, n=n: E.activation(out=sa[fc][:, :n], in_=self.psA[fc][:, :n], func=AF.Silu),
                             reads=[("psA", fc)], writes=[("m_sa", fc)])
                        if pend is not None:
                            pend(range(4 * fc, 4 * fc + 4))
                        P.op("dve", lambda E, fc=fc, n=n: E.tensor_tensor(out=sa[fc][:, :n], in0=sa[fc][:, :n],
                                                                          in1=self.psB[fc][:, :n], op=ALU.mult),
                             reads=[("m_sa", fc), ("psB", fc)], writes=[("m_sa", fc)])
                        P.op("dve", lambda E, fc=fc, n=n, hb=hb: E.tensor_tensor(out=hid[hb][:, fc, :n], in0=sa[fc][:, :n],
                                                                                 in1=self.psG[:, :n], op=ALU.mult),
                             reads=[("m_sa", fc), ("psG",)], writes=HK[hb])
                    if ti == 0:
                        emit_cast()
                        emit_cast()
                        if nt == 1:
                            for _ in range(4):
                                if e + 1 < NEXP:
                                    emit_cast()

                    def down(dcs, wb=wb, hb=hb, t0=t0, n=n, src=src, g2=g2, stream=stream):
                        for dc in dcs:
                            ob = obank[0] % 3
                            obank[0] += 1
                            po, pok = obanks[ob]

                            def dmm(E, dc=dc, po=po):
                                ins = None
                                for fc in range(2):
                                    ins = E.matmul(po[:, :n], lhsT=wdn[:, fc, dc * 128:(dc + 1) * 128],
                                                   rhs=hid[hb][:, fc, :n], start=(fc == 0), stop=(fc == 1))
                                return ins
                            P.op("pe", dmm, reads=[("wdn", 0), ("wdn", 1)] + HK[hb], writes=[pok])
                            if dc % 2 == 0:
                                P.op("dve", lambda E, dc=dc, po=po: E.scalar_tensor_tensor(
                                    out=src[:, dc, t0:t0 + n], in0=po[:, :n], scalar=g2[:, dc:dc + 1],
                                    in1=src[:, dc, t0:t0 + n], op0=ALU.mult, op1=ALU.add),
                                    reads=[pok] + mk + self.skeys(stream, [dc], t0, n),
                                    writes=self.skeys(stream, [dc], t0, n))
                            else:
                                P.op("act", lambda E, dc=dc, po=po: E.activation(out=rtmp[:, :n], in_=po[:, :n], func=AF.Identity,
                                                                                 scale=g2[:, dc:dc + 1]),
                                     reads=[pok] + mk, writes=[("rt",)])
                                P.op("pool", lambda E, dc=dc: E.tensor_tensor(out=src[:, dc, t0:t0 + n], in0=src[:, dc, t0:t0 + n],
                                                                              in1=rtmp[:, :n], op=ALU.add),
                                     reads=[("rt",)] + self.skeys(stream, [dc], t0, n), writes=self.skeys(stream, [dc], t0, n))
                    pend = down
                    it += 1
            if pend is not None:
                pend(range(NCH))
                pend = None

    def routing(self, l, ns, gT2, lo):
        P = self.P
        rt = self.rt
        o = [0]

        def take(nw, shape):
            a = rt[:, o[0]:o[0] + nw]
            o[0] += nw
            if len(shape) > 1:
                names = " ".join("a%d" % i for i in range(len(shape)))
                kw = {"a%d" % i: s for i, s in enumerate(shape)}
                a = a.rearrange("p (%s) -> p %s" % (names, names), **kw)
            return a
        lgs = take(80, [4, 20])[:, :ns]
        gmax = take(4, [4])[:, :ns]
        gmask = take(16, [4, 4])[:, :ns]
        gd = take(16, [4, 4])[:, :ns]
        gsum = take(4, [4])[:, :ns]
        gw = take(4, [4])[:, :ns]
        em = take(64, [4, 4, 4])[:, :ns]
        ein = take(16, [4, 4])[:, :ns]
        m1 = take(4, [4])[:, :ns]
        mask1 = take(16, [4, 4])[:, :ns]
        e2 = take(16, [4, 4])[:, :ns]
        m2 = take(4, [4])[:, :ns]
        mask2 = take(16, [4, 4])[:, :ns]
        dm = take(4, [4])[:, :ns]
        w1 = take(4, [4])[:, :ns]
        w2 = take(4, [4])[:, :ns]
        ew = take(16, [4, 4])[:, :ns]
        ew2 = take(16, [4, 4])[:, :ns]
        gates = take(64, [4, 16])[:, :ns]
        ghf = take(64, [4, 16])[:, :ns]
        g2t = take(64, [4 * 32]).bitcast(BF16).rearrange("p (s k) -> p s k", k=32)[:, :ns]
        assert o[0] <= 512
        K = ("rt",)
        bc3 = lambda a: a.unsqueeze(2).broadcast_to([128, ns, 4])
        glog = lgs[:, :, 0:4]
        elog = lgs[:, :, 4:20].rearrange("p s (g e) -> p s g e", g=4)

        def dve(fn, extra_r=()):
            P.op("dve", fn, reads=[K] + list(extra_r), writes=[K])

        P.op("dve", lambda E: E.tensor_tensor(out=lgs, in0=self.psG[:, 0:ns * 20].rearrange("p (s k) -> p s k", k=20),
                                              in1=self.br.unsqueeze(1).broadcast_to([128, ns, 20]), op=ALU.add),
             reads=[("psG",), ("br",), K], writes=[K])
        dve(lambda E: E.tensor_reduce(out=gmax, in_=glog, axis=AX.X, op=ALU.max))
        dve(lambda E: E.tensor_tensor(out=gmask, in0=glog, in1=bc3(gmax), op=ALU.is_ge))
        dve(lambda E: E.tensor_tensor(out=gd, in0=glog, in1=bc3(gmax), op=ALU.subtract))
        P.op("act", lambda E: E.activation(out=gd, in_=gd, func=AF.Exp), reads=[K], writes=[K])
        dve(lambda E: E.tensor_reduce(out=gsum, in_=gd, axis=AX.X, op=ALU.add))
        dve(lambda E: E.reciprocal(out=gw, in_=gsum))
        dve(lambda E: E.tensor_tensor(out=em, in0=elog, in1=gmask.unsqueeze(3).broadcast_to([128, ns, 4, 4]), op=ALU.mult))
        dve(lambda E: E.tensor_reduce(out=ein, in_=em.rearrange("p s g e -> p s e g"), axis=AX.X, op=ALU.add))
        dve(lambda E: E.tensor_reduce(out=m1, in_=ein, axis=AX.X, op=ALU.max))
        dve(lambda E: E.tensor_tensor(out=mask1, in0=ein, in1=bc3(m1), op=ALU.is_ge))
        dve(lambda E: E.scalar_tensor_tensor(out=e2, in0=mask1, scalar=-1e30, in1=ein, op0=ALU.mult, op1=ALU.add))
        dve(lambda E: E.tensor_reduce(out=m2, in_=e2, axis=AX.X, op=ALU.max))
        dve(lambda E: E.tensor_tensor(out=mask2, in0=e2, in1=bc3(m2), op=ALU.is_ge))
        dve(lambda E: E.tensor_tensor(out=dm, in0=m2, in1=m1, op=ALU.subtract))
        P.op("act", lambda E: E.activation(out=dm, in_=dm, func=AF.Exp), reads=[K], writes=[K])
        dve(lambda E: E.tensor_scalar(out=w1, in0=dm, scalar1=1.0, scalar2=None, op0=ALU.add))
        dve(lambda E: E.reciprocal(out=w1, in_=w1))
        dve(lambda E: E.tensor_tensor(out=w2, in0=dm, in1=w1, op=ALU.mult))
        dve(lambda E: E.tensor_tensor(out=w1, in0=w1, in1=gw, op=ALU.mult))
        dve(lambda E: E.tensor_tensor(out=w2, in0=w2, in1=gw, op=ALU.mult))
        dve(lambda E: E.tensor_tensor(out=ew, in0=mask1, in1=bc3(w1), op=ALU.mult))
        dve(lambda E: E.tensor_tensor(out=ew2, in0=mask2, in1=bc3(w2), op=ALU.mult))
        dve(lambda E: E.tensor_tensor(out=ew, in0=ew, in1=ew2, op=ALU.add))
        dve(lambda E: E.tensor_tensor(out=gates.rearrange("p s (g e) -> p s g e", g=4),
                                      in0=gmask.unsqueeze(3).broadcast_to([128, ns, 4, 4]),
                                      in1=ew.unsqueeze(2).broadcast_to([128, ns, 4, 4]), op=ALU.mult))
        dve(lambda E: E.tensor_copy(out=g2t[:, :, 0:16], in_=gates))
        dve(lambda E: E.tensor_copy(out=ghf, in_=g2t[:, :, 0:16]))
        dve(lambda E: E.tensor_tensor(out=g2t[:, :, 16:32], in0=gates, in1=ghf, op=ALU.subtract))
        psMb = self.psM.bitcast(BF16)

        def tr(E):
            ins = None
            for s in range(ns):
                ins = E.transpose(out=psMb[0:32, s * 128:(s + 1) * 128], in_=g2t[:, s, :], identity=self.identb)
            return ins
        P.op("pe", tr, reads=[K, ("identb",)], writes=[("psM",)])
        P.op("act", lambda E: E.activation(out=gT2[0:32, lo:lo + ns * 128], in_=psMb[0:32, 0:ns * 128], func=AF.Copy),
             reads=[("psM",)], writes=[("gT2", lo // 512)])

    def pool_mixer(self, l, pj, stream):
        P = self.P
        src, N = self.S(stream)
        j = 0 if stream == "x" else 1
        NHB = min(N, 2048)
        W = NHB + 16
        rstd = self.carve(0, 4096)
        hc = self.carve(4096, 2064)
        sA = self.carve(6160, 2064)
        sB = self.carve(8224, 2064)
        pT = self.carve(10288, 4096, BF16, [2, 4096])
        pwb = self.carve(14384, 1024, BF16, [4, 2, 256])
        sq = [self.carve(15408, 256, BF16), self.carve(15664, 256, BF16)]
        gs1 = self.gs[:, l, 0, j, :]
        sh1 = self.mod(l, 0, j)
        g1 = self.mod(l, 2, j)
        mk = self.modkeys(l)
        cs = self.pcoef[:, 0, :]
        cb = self.pcoef[:, 1, :]
        P.op("pool", lambda E: E.dma_start(out=pwb, in_=self.d_pw[pj]), writes=[("p_w",)], dma=True)
        P.op("dve", lambda E: E.tensor_tensor(out=cs, in0=g1, in1=self.pv[:, pj, 1, :], op=ALU.mult),
             reads=mk + [("pv",)], writes=[("pcoef",)])
        P.op("dve", lambda E: E.tensor_tensor(out=cb, in0=cs, in1=self.pv[:, pj, 0, :], op=ALU.mult),
             reads=[("pcoef",), ("pv",)], writes=[("pcoef",)])
        for t0 in range(0, N, 512):
            n = min(512, N - t0)
            self.rstd_tile(stream, t0, n, rstd[:, t0:t0 + n], ("p_rstd", t0 // 512), sq, [("p_sq0",), ("p_sq1",)])
        for g in range(4):
            w = 2 << g
            for kc in range(2):
                c = 2 * g + kc
                for T0 in range(0, N, NHB):
                    a = max(0, T0 - 8)
                    b = min(N, T0 + NHB + 8)
                    ca, cbb = a - T0 + 8, b - T0 + 8
                    if T0 == 0:
                        P.op("pool", lambda E: E.memset(hc[:, 0:8], 0.0), writes=[("p_hc",)])
                    if T0 + NHB == N:
                        P.op("pool", lambda E: E.memset(hc[:, 8 + NHB:16 + NHB], 0.0), writes=[("p_hc",)])
                    P.op("dve", lambda E, c=c, a=a, b=b, ca=ca, cbb=cbb: E.scalar_tensor_tensor(
                        out=hc[:, ca:cbb], in0=src[:, c, a:b], scalar=gs1[:, c:c + 1], in1=rstd[:, a:b],
                        op0=ALU.mult, op1=ALU.mult),
                        reads=self.skeys(stream, [c], a, b - a) + mk + [("p_rstd", t) for t in _tiles(a, b - a)],
                        writes=[("p_hc",)])
                    P.op("act", lambda E, c=c, ca=ca, cbb=cbb: E.activation(out=hc[:, ca:cbb], in_=hc[:, ca:cbb], func=AF.Identity,
                                                                             bias=sh1[:, c:c + 1], scale=1.0),
                         reads=[("p_hc",)] + mk, writes=[("p_hc",)])
                    bufs = [hc, sA, sB, sA, sB]
                    keys = [("p_hc",), ("p_sA",), ("p_sB",), ("p_sA",), ("p_sB",)]
                    lo_, hi_ = 0, W
                    for lev in range(g + 1):
                        sh = 1 << max(lev - 1, 0)
                        i_, o_ = bufs[lev], bufs[lev + 1]
                        if lev == 0:
                            nlo, nhi = lo_ + 1, hi_
                            P.op("pool", lambda E, i_=i_, o_=o_, nlo=nlo, nhi=nhi: E.tensor_tensor(
                                out=o_[:, nlo:nhi], in0=i_[:, nlo - 1:nhi - 1], in1=i_[:, nlo:nhi], op=ALU.add),
                                reads=[keys[lev]], writes=[keys[lev + 1]])
                        else:
                            nlo, nhi = lo_ + sh, hi_ - sh
                            P.op("pool", lambda E, i_=i_, o_=o_, nlo=nlo, nhi=nhi, sh=sh: E.tensor_tensor(
                                out=o_[:, nlo:nhi], in0=i_[:, nlo - sh:nhi - sh], in1=i_[:, nlo + sh:nhi + sh], op=ALU.add),
                                reads=[keys[lev]], writes=[keys[lev + 1]])
                        lo_, hi_ = nlo, nhi
                    Sb, Sk = bufs[g + 1], keys[g + 1]
                    assert lo_ <= 8 and hi_ >= 8 + NHB
                    eo = 144 + g * 16
                    if T0 == 0:
                        P.op("dve", lambda E, Sb=Sb, eo=eo: E.tensor_tensor(out=Sb[:, 8:16], in0=Sb[:, 8:16],
                                                                            in1=self.consts[:, eo:eo + 8], op=ALU.mult),
                             reads=[Sk, ("consts",)], writes=[Sk])
                    if T0 + NHB == N:
                        P.op("dve", lambda E, Sb=Sb, eo=eo: E.tensor_tensor(out=Sb[:, NHB:NHB + 8], in0=Sb[:, NHB:NHB + 8],
                                                                            in1=self.consts[:, eo + 8:eo + 16], op=ALU.mult),
                             reads=[Sk, ("consts",)], writes=[Sk])
                    P.op("dve", lambda E, Sb=Sb, kc=kc, T0=T0, w=w: E.scalar_tensor_tensor(
                        out=pT[:, kc, T0:T0 + NHB], in0=Sb[:, 8:8 + NHB], scalar=1.0 / w, in1=hc[:, 8:8 + NHB],
                        op0=ALU.mult, op1=ALU.subtract),
                        reads=[Sk, ("p_hc",)], writes=[("p_pT", kc, t) for t in _tiles(T0, NHB)])
            for t0 in range(0, N, 512):
                n = min(512, N - t0)
                for oc in range(2):
                    dc = 2 * g + oc
                    po = self.psO[oc]

                    def pmm(E, g=g, oc=oc, po=po, t0=t0, n=n):
                        ins = None
                        for kc in range(2):
                            ins = E.matmul(po[:, :n], lhsT=pwb[:, g, kc, oc * 128:(oc + 1) * 128], rhs=pT[:, kc, t0:t0 + n],
                                           start=(kc == 0), stop=(kc == 1))
                        return ins
                    P.op("pe", pmm, reads=[("p_w",), ("p_pT", 0, t0 // 512), ("p_pT", 1, t0 // 512)], writes=[("psO", oc)])
                    P.op("dve", lambda E, dc=dc, po=po, t0=t0, n=n: E.scalar_tensor_tensor(
                        out=src[:, dc, t0:t0 + n], in0=po[:, :n], scalar=cs[:, dc:dc + 1], in1=src[:, dc, t0:t0 + n],
                        op0=ALU.mult, op1=ALU.add),
                        reads=[("psO", oc), ("pcoef",)] + self.skeys(stream, [dc], t0, n), writes=self.skeys(stream, [dc], t0, n))
                    P.op("dve", lambda E, dc=dc, t0=t0, n=n: E.tensor_scalar(
                        out=src[:, dc, t0:t0 + n], in0=src[:, dc, t0:t0 + n], scalar1=cb[:, dc:dc + 1], scalar2=None, op0=ALU.add),
                        reads=[("pcoef",)] + self.skeys(stream, [dc], t0, n), writes=self.skeys(stream, [dc], t0, n))

    def headnorm_rope(self, ps_q, n, gvec, rope, tab, out_ap, out_keys, tmp):
        P = self.P
        obk, Rm = self.obk, self.Rm
        sq, rstd, qn, t1, psms, psms_key, psq_key = tmp["sq"], tmp["rstd"], tmp["qn"], tmp["t1"], tmp["psms"], tmp["psms_key"], tmp["psq_key"]
        P.op("act", lambda E: E.activation(out=sq[:, :n], in_=ps_q[:, :n], func=AF.Square),
             reads=[psq_key], writes=[("a_sq",)])
        P.op("pe", lambda E: E.matmul(psms[:, :n], lhsT=obk, rhs=sq[:, :n], start=True, stop=True),
             reads=[("a_sq",), ("a_const",)], writes=[psms_key])
        P.op("act", lambda E: E.activation(out=rstd[:, :n], in_=psms[:, :n], func=AF.Ln, bias=self.epst, scale=1.0),
             reads=[psms_key, ("epst",)], writes=[("a_rstd",)])
        P.op("act", lambda E: E.activation(out=rstd[:, :n], in_=rstd[:, :n], func=AF.Exp, scale=-0.5),
             reads=[("a_rstd",)], writes=[("a_rstd",)])
        outs = out_ap
        if not rope:
            (oa, r0, r1), = outs
            P.op("dve", lambda E: E.scalar_tensor_tensor(out=oa, in0=ps_q[:, :n], scalar=gvec, in1=rstd[:, :n],
                                                         op0=ALU.mult, op1=ALU.mult),
                 reads=[psq_key, ("a_rstd",), ("a_const",)], writes=out_keys)
            return
        P.op("dve", lambda E: E.scalar_tensor_tensor(out=qn[:, :n], in0=ps_q[:, :n], scalar=gvec, in1=rstd[:, :n],
                                                     op0=ALU.mult, op1=ALU.mult),
             reads=[psq_key, ("a_rstd",), ("a_const",)], writes=[("a_qn",)])
        P.op("pe", lambda E: E.matmul(ps_q[:, :n], lhsT=Rm, rhs=qn[:, :n], start=True, stop=True),
             reads=[("a_qn",), ("a_const",)], writes=[psq_key])
        P.op("pool", lambda E: E.tensor_tensor(out=t1[:, :n], in0=qn[:, :n], in1=tab[0][:, :n], op=ALU.mult),
             reads=[("a_qn",), ("a_tab",)], writes=[("a_t1",)])
        P.op("dve", lambda E: E.tensor_tensor(out=rstd[:, :n], in0=ps_q[:, :n], in1=tab[1][:, :n], op=ALU.mult),
             reads=[psq_key, ("a_tab",), ("a_rstd",)], writes=[("a_rstd",)])
        for (oa, r0, r1) in outs:
            P.op("pool", lambda E, oa=oa, r0=r0, r1=r1: E.tensor_tensor(out=oa, in0=t1[r0:r1, :n], in1=rstd[r0:r1, :n], op=ALU.add),
                 reads=[("a_t1",), ("a_rstd",)], writes=out_keys)

    def attn_norm(self, l, stream, t0, n, aT, sq2, rstd, ps=None, ps_key=None, all_pool=False):
        P = self.P
        src, _ = self.S(stream)
        j = 0 if stream == "x" else 1
        gs1 = self.gs[:, l, 0, j, :]
        sh1 = self.mod(l, 0, j)
        mk = self.modkeys(l)
        self.rstd_tile(stream, t0, n, rstd[:, :n], ("a_rstd",), sq2, [("a_sq",), ("a_qn",)], ps=ps, ps_key=ps_key,
                       all_pool=all_pool)
        a_tmp, a_tmpk = list(self.a_tmp), list(self.a_tmpk)
        for c in range(NCH):
            P.op("dve", lambda E, c=c: E.scalar_tensor_tensor(out=a_tmp[c % 2][:, :n], in0=src[:, c, t0:t0 + n],
                                                              scalar=gs1[:, c:c + 1], in1=rstd[:, :n], op0=ALU.mult, op1=ALU.mult),
                 reads=self.skeys(stream, [c], t0, n) + mk + [("a_rstd",)], writes=[a_tmpk[c % 2]])
            P.op("act", lambda E, c=c: E.activation(out=aT[:, c, :n], in_=a_tmp[c % 2][:, :n], func=AF.Identity,
                                                    bias=sh1[:, c:c + 1], scale=1.0),
                 reads=[a_tmpk[c % 2]] + mk, writes=[("a_aT", c)])

    def attn_mixer(self, l):
        P = self.P
        NKT = 34
        KT = self.carve(0, 4352, BF16, [2, 4352])
        V = self.carve(4352, 4352, BF16, [NKT, 256])
        aT = self.carve(8704, 2048, BF16, [NCH, 512])
        tab = [self.carve(10752, 512), self.carve(11264, 512)]
        wq = self.carve(11776, 512, BF16, [NCH, 128])
        wkv = self.carve(11776, 2048, BF16, [NCH, 512])
        QTp = [self.carve(12288, 256, BF16), self.carve(12544, 256, BF16)]
        oT = self.carve(12800, 512, BF16, [2, 512])
        sTf = self.sT.rearrange("p c t -> p (c t)")
        PT = [self.carve(13312, 256, BF16), self.carve(13568, 256, BF16),
              sTf[:, 1024:1280].bitcast(BF16), sTf[:, 1280:1536].bitcast(BF16)]
        sq = self.carve(13824, 256, BF16)
        rstd = self.carve(14080, 512)
        qn = self.carve(14592, 256, BF16)
        t1 = self.carve(14848, 512)
        self.obk = self.carve(15360, 64, BF16)
        self.Rm = self.carve(15424, 64, BF16)
        ones128 = self.carve(15488, 128)
        rec = self.carve(15616, 512)
        ones128b = self.carve(16128, 64, BF16)
        self.a_tmp = [t1, tab[0]]
        self.a_tmpk = [("a_t1",), ("a_tab",)]
        woG = self.sT.rearrange("p c t -> p (c t)")[:, 0:1024].bitcast(BF16).rearrange("p (s d) -> p s d", s=2)
        g1 = self.mod(l, 2, 0)
        mk = self.modkeys(l)
        psQ, psMS = self.psA[0], self.psB[0]
        psS = [self.psA[1], self.psB[1]]
        psOut = [self.psO[0], self.psG]
        psDen = [self.psO[1], self.psM]
        kOut = [("psO", 0), ("psG",)]
        kDen = [("psO", 1), ("psM",)]
        psY = [psQ, psMS]
        kY = [("psA", 0), ("psB", 0)]
        tmp = dict(sq=sq, rstd=rstd, qn=qn, t1=t1, psms=psMS, psms_key=("psB", 0), psq_key=("psA", 0))
        P.op("pool", lambda E: E.memset(self.obk, 0.0), writes=[("a_const",)])
        P.op("pool", lambda E: E.memset(self.obk[0:64, 0:64], 1.0 / 64), writes=[("a_const",)])
        P.op("pool", lambda E: E.memset(self.obk[64:128, 64:128], 1.0 / 64), writes=[("a_const",)])
        idv = self.ident.rearrange("p (b h k) -> p b h k", h=2, k=16)
        rmv = self.Rm.rearrange("p (b h k) -> p b h k", h=2, k=16)
        P.op("dve", lambda E: E.tensor_scalar(out=rmv[:, :, 0, :], in0=idv[:, :, 1, :], scalar1=-1.0, scalar2=None, op0=ALU.mult),
             reads=[("consts",)], writes=[("a_const",)])
        P.op("dve", lambda E: E.tensor_copy(out=rmv[:, :, 1, :], in_=idv[:, :, 0, :]), reads=[("consts",)], writes=[("a_const",)])
        P.op("pool", lambda E: E.memset(ones128, 1.0), writes=[("a_const",)])
        P.op("pool", lambda E: E.memset(ones128b, 1.0), writes=[("a_const",)])
        P.op("sp", lambda E: E.dma_start(out=self.av, in_=self.d_av), writes=[("a_av",)], dma=True)
        P.op("dve", lambda E: E.tensor_scalar(out=self.av[:, 0:1], in0=self.av[:, 0:1], scalar1=0.125, scalar2=None, op0=ALU.mult),
             reads=[("a_av",)], writes=[("a_const",)])
        P.op("pool", lambda E: E.dma_start(out=wkv, in_=self.d_wkv), writes=[("a_wkv",)], dma=True)
        segs = [("s", 0, CTXN, 0, False)] + [("x", t0, 512, CTXN + t0, True) for t0 in range(0, SEQ, 512)]
        for (stream, t0, n, k0, rope) in segs:
            self.attn_norm(l, stream, t0, n, aT, [sq, qn], rstd)
            if rope:
                for i in range(2):
                    P.op("sp", lambda E, i=i, t0=t0, n=n: E.dma_start(out=tab[i][:, :n], in_=self.d_rope[i, :, t0:t0 + n]),
                         writes=[("a_tab",)], dma=True)
            for kc in range(2):
                def kmm(E, kc=kc, n=n):
                    ins = None
                    for k in range(NCH):
                        ins = E.matmul(psQ[:, :n], lhsT=wkv[:, k, kc * 128:(kc + 1) * 128], rhs=aT[:, k, :n],
                                       start=(k == 0), stop=(k == NCH - 1))
                    return ins
                P.op("pe", kmm, reads=[("a_wkv",)] + [("a_aT", c) for c in range(NCH)], writes=[("psA", 0)])
                self.headnorm_rope(psQ, n, self.av[:, 1:2], rope, tab, [(KT[:, kc, k0:k0 + n], 0, 128)],
                                   [("a_KT", kc, kt) for kt in range(k0 // 128, (k0 + n) // 128)], tmp)
            for sub in range(n // 128):
                kt = k0 // 128 + sub

                def vmm(E, sub=sub):
                    ins = None
                    for k in range(NCH):
                        ins = E.matmul(psMS[:, 0:256], lhsT=aT[:, k, sub * 128:(sub + 1) * 128], rhs=wkv[:, k, 256:512],
                                       start=(k == 0), stop=(k == NCH - 1))
                    return ins
                P.op("pe", vmm, reads=[("a_wkv",)] + [("a_aT", c) for c in range(NCH)], writes=[("psB", 0)])
                P.op("act", lambda E, kt=kt: E.activation(out=V[:, kt, :], in_=psMS[:, 0:256], func=AF.Copy),
                     reads=[("psB", 0)], writes=[("a_V", kt)])
        P.barrier()
        QT2 = [QTp, [sTf[:, 1536:1792].bitcast(BF16), sTf[:, 1792:2048].bitcast(BF16)]]
        for par in range(2):
            for half in range(2):
                P.op("pool", lambda E, par=par, half=half: E.memset(QT2[par][half], 0.0), writes=[("a_QT", par, half)])

        def make_q(c):
            par = c % 2
            P.begin_capture()
            P.op("pool", lambda E: E.dma_start(out=wq, in_=self.d_wq[c]), writes=[("a_wq",)], dma=True)

            def qmm(E):
                ins = None
                for k in range(NCH):
                    ins = E.matmul(psQ, lhsT=wq[:, k, :], rhs=aT[:, k, :], start=(k == 0), stop=(k == NCH - 1))
                return ins
            P.op("pe", qmm, reads=[("a_wq",)] + [("a_aT", cc) for cc in range(NCH)], writes=[("psA", 0)])
            self.headnorm_rope(psQ, 512, self.av[:, 0:1], True, tab,
                               [(QT2[par][0][0:64, :], 0, 64), (QT2[par][1][64:128, :], 64, 128)],
                               [("a_QT", par, 0), ("a_QT", par, 1)], tmp)
            return P.end_capture()
        hn = 0
        pending_wo = []
        tail = []
        NQT = SEQ // 512

        def qtile_prep(qt_, deferred_mode):
            t0_ = qt_ * 512
            if deferred_mode:
                P.begin_capture()
                self.attn_norm(l, "x", t0_, 512, aT, [sq, qn], rstd, ps=psMS, ps_key=("psB", 0), all_pool=True)
            else:
                self.attn_norm(l, "x", t0_, 512, aT, [sq, qn], rstd)
            for i in range(2):
                P.op("sp", lambda E, i=i: E.dma_start(out=tab[i], in_=self.d_rope[i, :, t0_:t0_ + 512]),
                     writes=[("a_tab",)], dma=True)
            ops = P.end_capture() if deferred_mode else []
            return ops + make_q(0)
        for o_ in qtile_prep(0, False):
            P.emit(o_)
        for qt in range(NQT):
            t0 = qt * 512
            n = 512
            for c in range(NCH):
                cp, ci = divmod(c, 2)
                kc = c // 4
                par = c % 2
                if c == 0 and qt == 0:
                    P.op("pool", lambda E, cp=cp: E.dma_start(out=woG, in_=self.d_wo[cp]), writes=[("a_wo",)], dma=True)
                if c + 1 < NCH:
                    dq = make_q(c + 1)
                elif qt + 1 < NQT:
                    dq = qtile_prep(qt + 1, True)
                else:
                    dq = []
                deferred = pending_wo + dq
                n_wo = len(pending_wo)
                pending_wo = []
                slot = 0
                for half in range(2):
                    r0, r1 = half * 64, half * 64 + 64
                    po, pd = psOut[hn % 2], psDen[hn % 2]
                    pok, pdk = kOut[hn % 2], kDen[hn % 2]
                    hn += 1
                    qtb, qtk = QT2[par][half], ("a_QT", par, half)
                    pe_all = False

                    def pv(kt, kc=kc, po=po, pd=pd, pok=pok, pdk=pdk, pe_all=pe_all):
                        pt, ptk = PT[kt % 4], ("a_PT", kt % 4)
                        P.op("pe", lambda E: E.matmul(po, lhsT=V[:, kt, kc * 128:(kc + 1) * 128], rhs=pt,
                                                      start=(kt == 0), stop=(kt == NKT - 1)),
                             reads=[("a_V", kt), ptk], writes=[pok])
                        if pe_all or kt % 2 == 1 or kt < 6:
                            P.op("pe", lambda E: E.matmul(pd, lhsT=ones128b, rhs=pt, start=(kt == 0),
                                                          stop=(pe_all and kt == NKT - 1)),
                                 reads=[("a_const",), ptk], writes=[pdk])
                        elif kt == 6:
                            P.op("dve", lambda E: E.tensor_copy(out=rec, in_=pt), reads=[ptk], writes=[("a_rec",)])
                        else:
                            P.op("dve", lambda E: E.tensor_tensor(out=rec, in0=rec, in1=pt, op=ALU.add),
                                 reads=[ptk, ("a_rec",)], writes=[("a_rec",)])
                    for kt in range(NKT):
                        P.op("pe", lambda E, kt=kt, kc=kc, qtb=qtb: E.matmul(
                            psS[kt % 2], lhsT=KT[:, kc, kt * 128:(kt + 1) * 128], rhs=qtb, start=True, stop=True),
                            reads=[("a_KT", kc, kt), qtk], writes=[("psS", kt % 2)])
                        P.op("act", lambda E, kt=kt: E.activation(out=PT[kt % 4], in_=psS[kt % 2], func=AF.Exp),
                             reads=[("psS", kt % 2)], writes=[("a_PT", kt % 4)])
                        if tail and kt >= 1:
                            P.emit(tail.pop(0))
                        if kt > 1:
                            pv(kt - 2)
                        slot += 1
                        wo_left = max(0, len(deferred) - len(dq))
                        if deferred and not tail and (wo_left == 0 or kt >= 10):
                            if wo_left > 0 or slot % 2 == 0 or 2 * len(deferred) > (2 * NKT - slot):
                                P.emit(deferred.pop(0))
                    pv(NKT - 2)
                    pv(NKT - 1)
                    while tail:
                        P.emit(tail.pop(0))
                    if half == 0:
                        while len(deferred) > len(dq):
                            P.emit(deferred.pop(0))
                    P.begin_capture()
                    if not pe_all:
                        P.op("pe", lambda E, pd=pd: E.matmul(pd, lhsT=ones128, rhs=rec, start=False, stop=True),
                             reads=[("a_const",), ("a_rec",)], writes=[pdk])
                    for qq in range(4):
                        P.op("dve", lambda E, pd=pd, r0=r0, r1=r1, qq=qq: E.reciprocal(
                            out=rec[r0:r1, qq * 128:(qq + 1) * 128], in_=pd[r0:r1, qq * 128:(qq + 1) * 128]),
                            reads=[pdk], writes=[("a_rec",)])
                    P.op("dve", lambda E, po=po, ci=ci, r0=r0, r1=r1: E.tensor_tensor(
                        out=oT[r0:r1, ci, :], in0=po[r0:r1, :], in1=rec[r0:r1, :], op=ALU.mult),
                        reads=[pok, ("a_rec",)], writes=[("a_oT", ci)])
                    tail = P.end_capture()
                    if c == NCH - 1 and half == 1 and qt == NQT - 1:
                        while tail:
                            P.emit(tail.pop(0))
                for o_ in deferred:
                    P.emit(o_)
                if ci == 1:
                    P.begin_capture()
                    for dm in range(NCH):
                        py, pyk = psY[dm % 2], kY[dm % 2]

                        def omm(E, dm=dm, py=py):
                            ins = None
                            for cj in range(2):
                                ins = E.matmul(py, lhsT=woG[:, cj, dm * 128:(dm + 1) * 128], rhs=oT[:, cj, :],
                                               start=(cj == 0), stop=(cj == 1))
                            return ins
                        P.op("pe", omm, reads=[("a_wo",), ("a_oT", 0), ("a_oT", 1)], writes=[pyk])
                        P.op("dve", lambda E, dm=dm, py=py, t0=t0: E.scalar_tensor_tensor(
                            out=self.xT[:, dm, t0:t0 + 512], in0=py, scalar=g1[:, dm:dm + 1], in1=self.xT[:, dm, t0:t0 + 512],
                            op0=ALU.mult, op1=ALU.add),
                            reads=[pyk] + mk + self.skeys("x", [dm], t0, 512), writes=self.skeys("x", [dm], t0, 512))
                    if cp + 1 < 4 or qt + 1 < NQT:
                        P.op("pool", lambda E, cp=cp: E.dma_start(out=woG, in_=self.d_wo[(cp + 1) % 4]), writes=[("a_wo",)], dma=True)
                    pending_wo = P.end_capture()
                    if cp == 3 and qt == NQT - 1:
                        for o_ in pending_wo:
                            P.emit(o_)
                        pending_wo = []

    def sgu_mixer(self, l):
        P = self.P
        winv = self.carve(0, 4096, BF16, [NCH, 1024])
        wsT = self.carve(4096, 512, BF16, [8, 128])
        Bg = self.carve(4608, 1024, F32, [8, 128])
        aT = self.carve(5632, 2048, BF16, [NCH, 512])
        uv = self.carve(7680, 2048, BF16, [NCH, 512])
        v = self.carve(9728, 1024)
        vn = self.carve(10752, 512, BF16)
        wu = [self.carve(11264, 512, BF16, [NCH, 128]), self.carve(11776, 512, BF16, [NCH, 128])]
        wo = [self.carve(12288, 512, BF16, [NCH, 128]), self.carve(12800, 512, BF16, [NCH, 128])]
        tmpa = self.carve(13568, 512)
        tmpb = self.carve(14080, 512)
        lbb = self.carve(13568, 512, BF16)
        sq0 = self.carve(14592, 256, BF16)
        sq1 = self.carve(14848, 256, BF16)
        rstd = self.carve(15104, 512)
        binv = self.carve(15616, 512, BF16)
        ones1 = self.carve(16128, 64, BF16)
        st = self.carve(16192, 16)
        sgv = self.carve(16208, 16, F32, [2, NCH])
        tsm = self.carve(16224, 128)
        self.a_tmp = [tmpa, tmpb]
        self.a_tmpk = [("a_t1",), ("a_tab",)]
        g1 = self.mod(l, 2, 0)
        mk = self.modkeys(l)
        P.op("pool", lambda E: E.dma_start(out=winv, in_=self.d_winv), writes=[("g_winv",)], dma=True)
        P.op("pool", lambda E: E.dma_start(out=wsT, in_=self.d_wsT), writes=[("g_ws",)], dma=True)
        P.op("pool", lambda E: E.dma_start(out=lbb, in_=self.d_lnb), writes=[("a_t1",)], dma=True)
        P.op("pool", lambda E: E.dma_start(out=binv[0:1, :], in_=self.d_binv), writes=[("g_binv",)], dma=True)
        P.op("sp", lambda E: E.dma_start(out=Bg, in_=self.d_bsb), writes=[("g_Bg",)], dma=True)
        P.op("sp", lambda E: E.dma_start(out=sgv, in_=self.d_sgv), writes=[("g_sgv",)], dma=True)
        P.op("pool", lambda E: E.memset(ones1, 1.0), writes=[("g_ones",)])
        for g in range(8):
            P.op("pe", lambda E, g=g: E.matmul(self.psA[0][:, 0:128], lhsT=lbb[:, g * 128:(g + 1) * 128], rhs=wsT[:, g, :],
                                               start=True, stop=True),
                 reads=[("a_t1",), ("g_ws",)], writes=[("psA", 0)])
            P.op("dve", lambda E, g=g: E.tensor_tensor(out=Bg[:, g, :], in0=self.psA[0][:, 0:128], in1=Bg[:, g, :], op=ALU.add),
                 reads=[("psA", 0), ("g_Bg",)], writes=[("g_Bg",)])
        P.barrier()
        nw = [0, 0]
        for t0 in range(0, SEQ, 512):
            n = 512
            self.attn_norm(l, "x", t0, n, aT, [sq0, sq1], rstd)
            for g in range(8):
                wb = nw[0] % 2
                nw[0] += 1
                P.op("pool", lambda E, g=g, wb=wb: E.dma_start(out=wu[wb], in_=self.d_winu[g]), writes=[("g_wu", wb)], dma=True)
                pu = self.psA[g % 2]

                def umm(E, wb=wb, pu=pu):
                    ins = None
                    for k in range(NCH):
                        ins = E.matmul(pu, lhsT=wu[wb][:, k, :], rhs=aT[:, k, :], start=(k == 0), stop=(k == NCH - 1))
                    return ins
                P.op("pe", umm, reads=[("g_wu", wb)] + [("a_aT", c) for c in range(NCH)], writes=[("psA", g % 2)])
                P.op("act", lambda E, g=g, pu=pu: E.activation(out=uv[:, g, :], in_=pu, func=AF.Gelu_apprx_tanh,
                                                               bias=sgv[:, 0, g:g + 1], scale=1.0),
                     reads=[("psA", g % 2), ("g_sgv",)], writes=[("g_uv", g)])
            for ch in range(4):
                for fb in range(2):
                    pv = self.psB[fb]

                    def vmm(E, ch=ch, fb=fb, pv=pv):
                        E.matmul(pv, lhsT=ones1[0:1, :], rhs=binv[0:1, fb * 512:(fb + 1) * 512], start=True, stop=False)
                        ins = None
                        for k in range(NCH):
                            ins = E.matmul(pv, lhsT=aT[:, k, ch * 128:(ch + 1) * 128], rhs=winv[:, k, fb * 512:(fb + 1) * 512],
                                           start=False, stop=(k == NCH - 1))
                        return ins
                    P.op("pe", vmm, reads=[("g_winv",), ("g_binv",), ("g_ones",)] + [("a_aT", c) for c in range(NCH)],
                         writes=[("psB", fb)])
                    P.op("act", lambda E, fb=fb, pv=pv: E.activation(out=v[:, fb * 512:(fb + 1) * 512], in_=pv, func=AF.Gelu_apprx_tanh,
                                                                     accum_out=st[:, fb:fb + 1]),
                         reads=[("psB", fb)], writes=[("g_v", fb), ("g_st",)])
                P.op("act", lambda E: E.activation(out=vn, in_=v, func=AF.Square, accum_out=st[:, 2:3]),
                     reads=[("g_v", 0), ("g_v", 1)], writes=[("g_vn",), ("g_st",)])
                K = [("g_st",)]
                P.op("dve", lambda E: E.tensor_tensor(out=st[:, 3:4], in0=st[:, 0:1], in1=st[:, 1:2], op=ALU.add), reads=K, writes=K)
                P.op("dve", lambda E: E.tensor_scalar(out=st[:, 3:4], in0=st[:, 3:4], scalar1=1.0 / 1024, scalar2=None, op0=ALU.mult),
                     reads=K, writes=K)
                P.op("dve", lambda E: E.tensor_tensor(out=st[:, 4:5], in0=st[:, 3:4], in1=st[:, 3:4], op=ALU.mult), reads=K, writes=K)
                P.op("dve", lambda E: E.scalar_tensor_tensor(out=st[:, 5:6], in0=st[:, 2:3], scalar=1.0 / 1024, in1=st[:, 4:5],
                                                             op0=ALU.mult, op1=ALU.subtract), reads=K, writes=K)
                P.op("act", lambda E: E.activation(out=st[:, 6:7], in_=st[:, 5:6], func=AF.Ln, bias=self.epst, scale=1.0),
                     reads=K + [("epst",)], writes=K)
                P.op("act", lambda E: E.activation(out=st[:, 6:7], in_=st[:, 6:7], func=AF.Exp, scale=-0.5), reads=K, writes=K)
                P.op("dve", lambda E: E.scalar_tensor_tensor(out=st[:, 7:8], in0=st[:, 3:4], scalar=-1.0, in1=st[:, 6:7],
                                                             op0=ALU.mult, op1=ALU.mult), reads=K, writes=K)
                P.op("act", lambda E: E.activation(out=vn, in_=v, func=AF.Identity, bias=st[:, 7:8], scale=st[:, 6:7]),
                     reads=K + [("g_v", 0), ("g_v", 1)], writes=[("g_vn",)])
                for gb in range(2):
                    psv = self.psO[gb]

                    def smm(E, gb=gb, psv=psv):
                        ins = None
                        for gi in range(4):
                            g = gb * 4 + gi
                            ins = E.matmul(psv[:, gi * 128:(gi + 1) * 128], lhsT=vn[:, g * 128:(g + 1) * 128], rhs=wsT[:, g, :],
                                           start=True, stop=True)
                        return ins
                    P.op("pe", smm, reads=[("g_vn",), ("g_ws",)], writes=[("psO", gb)])
                    for gi in range(4):
                        g = gb * 4 + gi
                        P.op("dve", lambda E, g=g, gi=gi, psv=psv: E.scalar_tensor_tensor(
                            out=tsm, in0=psv[:, gi * 128:(gi + 1) * 128], scalar=sgv[:, 1, g:g + 1], in1=Bg[:, g, :],
                            op0=ALU.mult, op1=ALU.add),
                            reads=[("psO", gb), ("g_sgv",), ("g_Bg",)], writes=[("g_tsm",)])
                        P.op("dve", lambda E, g=g, ch=ch: E.tensor_tensor(out=uv[:, g, ch * 128:(ch + 1) * 128], in0=tsm,
                                                                          in1=uv[:, g, ch * 128:(ch + 1) * 128], op=ALU.mult),
                             reads=[("g_tsm",), ("g_uv", g)], writes=[("g_uv", g)])
            for dm in range(NCH):
                wb = nw[1] % 2
                nw[1] += 1
                P.op("pool", lambda E, dm=dm, wb=wb: E.dma_start(out=wo[wb], in_=self.d_wout[dm]), writes=[("g_wo", wb)], dma=True)
                py = self.psG if dm % 2 == 0 else self.psM
                pyk = ("psG",) if dm % 2 == 0 else ("psM",)

                def omm(E, wb=wb, py=py):
                    ins = None
                    for k in range(NCH):
                        ins = E.matmul(py, lhsT=wo[wb][:, k, :], rhs=uv[:, k, :], start=(k == 0), stop=(k == NCH - 1))
                    return ins
                P.op("pe", omm, reads=[("g_wo", wb)] + [("g_uv", g) for g in range(8)], writes=[pyk])
                P.op("dve", lambda E, dm=dm, py=py, t0=t0: E.scalar_tensor_tensor(
                    out=self.xT[:, dm, t0:t0 + 512], in0=py, scalar=g1[:, dm:dm + 1], in1=self.xT[:, dm, t0:t0 + 512],
                    op0=ALU.mult, op1=ALU.add),
                    reads=[pyk] + mk + self.skeys("x", [dm], t0, 512), writes=self.skeys("x", [dm], t0, 512))

    def epilogue(self, store_s=False):
        P = self.P
        for c in range(NCH):
            for h in range(2):
                P.op("sp", lambda E, c=c, h=h: E.dma_start(out=self.d_out[c, :, h * 2048:(h + 1) * 2048],
                                                           in_=self.xT[:, c, h * 2048:(h + 1) * 2048]),
                     reads=self.skeys("x", [c], h * 2048, 2048), dma=True, final=True)
            if self.store_s:
                P.op("sp", lambda E, c=c: E.dma_start(out=self.d_sout[c], in_=self.sT[:, c, :]),
                     reads=self.skeys("s", [c], 0, CTXN), dma=True, final=True)

    def mixer(self, li, l):
        kind = l % 3
        if kind == 0:
            self.pool_mixer(li, l // 3, "x")
            self.P.barrier()
            if l < 1:
                self.pool_mixer(li, l // 3, "s")
                self.P.barrier()
        elif kind == 1:
            self.attn_mixer(li)
            self.P.barrier()
        else:
            self.sgu_mixer(li)
            self.P.barrier()

    def build(self):
        self.prologue()
        self.P.barrier()
        for li, l in enumerate(self.layers):
            ph = self.phases
            if ph is None or ("mix", l) in ph:
                self.mixer(li, l)
            if ph is None:
                self.moe(li, "xs" if l < 1 else "x")
                self.P.barrier()
            else:
                if ("moe", l) in ph and ("moe_s", l) in ph:
                    self.moe(li, "xs")
                    self.P.barrier()
                elif ("moe", l) in ph:
                    self.moe(li, "x")
                    self.P.barrier()
                elif ("moe_s", l) in ph:
                    self.moe(li, "s")
                    self.P.barrier()
        self.epilogue()
        self.P.finalize()
        return self.nc


def make_consts():
    c = np.zeros((128, 256), np.float32)
    c[:, 0:128] = np.eye(128, dtype=np.float32)
    for k in range(32):
        c[k, 128 + (k % 16)] = 1.0
    for g in range(4):
        w = 2 << g
        for jj in range(8):
            cl = (jj + w // 2) - max(jj - w // 2, 0)
            cr = min(w // 2, 8 - jj) + w // 2
            c[:, 144 + g * 16 + jj] = w / cl
            c[:, 144 + g * 16 + 8 + jj] = w / cr
    return c


def rope_tables():
    t = np.arange(SEQ)
    inv = 10000.0 ** (-np.arange(0, 32, 2, dtype=np.float64) / 32)
    tab = np.zeros((2, 128, SEQ), np.float64)
    for p in range(128):
        d = p % 64
        pos = (t // 64) if d < 32 else (t % 64)
        ang = pos * np.float64(np.float32(inv[d % 16]))
        ang = np.float32(pos.astype(np.float32) * np.float32(inv[d % 16])).astype(np.float64)
        tab[0, p] = np.cos(ang)
        tab[1, p] = np.sin(ang)
    return tab.astype(np.float32)


def fm(v):
    v = np.asarray(v)
    lead = v.shape[:-1]
    return np.ascontiguousarray(np.moveaxis(v.reshape(lead + (NCH, 128)), -1, 0))


def prep_shared(inp, layers):
    L = len(layers)
    sh = {}
    sh["consts"] = make_consts()
    wm = inp["w_mod"][layers]
    sh["wmod"] = np.ascontiguousarray(wm.reshape(L, NCH, 128, 12, 512).transpose(0, 3, 2, 1, 4))
    sh["bmod"] = np.ascontiguousarray(inp["b_mod"][layers].reshape(L, 48, 128).transpose(2, 0, 1))
    ng = np.stack([inp["norm1_g"][layers], inp["norm2_g"][layers]], axis=1)
    sh["ng"] = fm(ng)
    wr = np.concatenate([inp["moe_wrg"][layers], inp["moe_wre"][layers]], axis=-1)
    sh["wr"] = np.ascontiguousarray(wr.reshape(L, NCH, 128, 20).transpose(2, 0, 1, 3))
    br = np.concatenate([inp["moe_brg"][layers], inp["moe_bre"][layers]], axis=-1)
    sh["br"] = np.ascontiguousarray(np.broadcast_to(br[None], (128, L, 20)))
    wgu = np.concatenate([inp["moe_wg"][layers], inp["moe_wu"][layers]], axis=-1)
    sh["wgu"] = np.ascontiguousarray(wgu.reshape(L, NEXP, NCH, 128, 512).transpose(0, 1, 3, 2, 4))
    wd = inp["moe_wd"][layers]
    sh["wd"] = np.ascontiguousarray(wd.reshape(L, NEXP, 2, 128, 1024).transpose(0, 1, 3, 2, 4))
    pw = inp["pool_w"]
    sh["pw"] = np.ascontiguousarray(pw.reshape(2, 4, 2, 128, 256).transpose(0, 3, 1, 2, 4))
    pv = np.stack([inp["pool_b"].reshape(2, D), inp["pool_ls"]], axis=1)
    sh["pv"] = fm(pv)
    heads = [(c, 4 + c) if c < 4 else (8 + c - 4, 12 + c - 4) for c in range(NCH)]
    wqkv = inp["attn_wqkv"][0]
    wqh = wqkv[:, :1024].reshape(D, 16, 64)
    wq = np.stack([np.concatenate([wqh[:, a], wqh[:, b]], axis=-1) for a, b in heads], axis=0)
    sh["wq"] = np.ascontiguousarray(wq.reshape(NCH, NCH, 128, 128).transpose(0, 2, 1, 3))
    sh["wkv"] = np.ascontiguousarray(wqkv[:, 1024:].reshape(NCH, 128, 512).transpose(1, 0, 2))
    woh = inp["attn_wo"][0].reshape(16, 64, D)
    order = [h for ab in heads for h in ab]
    sh["wo"] = np.ascontiguousarray(woh[order].reshape(4, 2, 128, D).transpose(0, 2, 1, 3))
    win = inp["sgu_win"][0]
    sh["winu"] = np.ascontiguousarray(win[:, :1024].reshape(NCH, 128, NCH, 128).transpose(2, 1, 0, 3))
    sh["winv"] = np.ascontiguousarray(win[:, 1024:].reshape(NCH, 128, 1024).transpose(1, 0, 2))
    sh["wout"] = np.ascontiguousarray(inp["sgu_wout"][0].reshape(NCH, 128, NCH, 128).transpose(2, 1, 0, 3))
    sh["sgv"] = fm(np.stack([inp["sgu_bin"][0][:1024], inp["sgu_lng"][0]], axis=0))
    sh["binv"] = np.ascontiguousarray(inp["sgu_bin"][0][1024:][None])
    sh["lnb"] = np.ascontiguousarray(np.broadcast_to(inp["sgu_lnb"][0][None], (128, 1024)))
    sh["bsb"] = np.ascontiguousarray(np.broadcast_to(inp["sgu_bs"][0][None], (128, 8, 128)))
    sh["wsT"] = np.ascontiguousarray(inp["sgu_ws"][0].transpose(2, 0, 1))
    sh["rope"] = rope_tables()
    sh["av"] = np.ascontiguousarray(np.stack([np.tile(inp["attn_qg"][0], 2), np.tile(inp["attn_kg"][0], 2)], axis=-1))
    return sh


def prep_core(inp, b):
    pc = {}
    pc["xT"] = np.ascontiguousarray(inp["x"][b].T.reshape(NCH, 128, SEQ))
    pc["sT"] = np.ascontiguousarray(inp["ctx"][b].T.reshape(NCH, 128, CTXN))
    cc = np.stack([inp["c"][b], inp["c_ctx"]], axis=-1)
    pc["ccT"] = np.ascontiguousarray(cc.reshape(NCH, 128, 2).transpose(1, 0, 2))
    return pc


def kernel(**inputs):
    inp = {k: np.asarray(v) for k, v in inputs.items()}
    layers = list(range(DEPTH))
    nc = bass.Bass("TRN2", target_bir_lowering=False)
    Builder(nc, layers, store_s=False).build()
    sh = prep_shared(inp, layers)
    in_maps = []
    for b in range(8):
        m = dict(sh)
        m.update(prep_core(inp, b))
        in_maps.append(m)
    res = run_bass_kernel_spmd(nc, in_maps, core_ids=list(range(8)))
    out = np.stack([r["yT"].reshape(D, SEQ).T for r in res.results], axis=0)
    return np.ascontiguousarray(out.astype(np.float32))
```
